# Optimizing a Trainium2 kernel written in Bass

```python
import jax
import jax.numpy as jnp
from jax import lax
import numpy as np

D_MODEL = 1024
BATCH = 8
SEQ = 8192
DEPTH = 2

HEAD_DIM = 64
MIX_WIDTH = D_MODEL
A_WIDTH = D_MODEL // 4
A_HEADS = A_WIDTH // HEAD_DIM
A_DK = HEAD_DIM
A_DV = HEAD_DIM
B_WIDTH = D_MODEL // 2
B_BLOCKS = B_WIDTH // HEAD_DIM
B_CONV = 4
LRU_C = 8.0
C_WIDTH = MIX_WIDTH - A_WIDTH - B_WIDTH
C_HEADS = C_WIDTH // HEAD_DIM
C_DV = HEAD_DIM
C_DK = C_DV // 2
C_GATE_RANK = 16
C_GATE_NORM = 16.0
CHUNK = 64
D_FF = 2688
FFN_CONV = 3
EPS = 1e-6
F_FLOOR = 1e-20

IN_SIZES = (A_HEADS * A_DK, A_HEADS * A_DK, A_HEADS * A_DV, A_HEADS * A_DV,
            B_WIDTH, B_WIDTH,
            C_HEADS * C_DK, C_HEADS * C_DK, C_WIDTH, C_WIDTH, C_GATE_RANK)
IN_SPLITS = tuple(int(s) for s in np.cumsum(IN_SIZES)[:-1])
IN_COLS = int(sum(IN_SIZES))

kernel_name = 'hymba_style_hgrn2_rglru_gla_convglu'


def rmsnorm(x, w):
    xf = x.astype(jnp.float32)
    y = xf * lax.rsqrt(jnp.mean(xf * xf, axis=-1, keepdims=True) + EPS)
    return (y * w.astype(jnp.float32)).astype(x.dtype)


def head_rmsnorm(o, w):
    b, t, h, d = o.shape
    return rmsnorm(o, w.reshape(h, d)).reshape(b, t, h * d)


def causal_dwconv(x, w, bias):
    k_w = w.shape[0]
    t = x.shape[1]
    xp = jnp.pad(x, ((0, 0), (k_w - 1, 0), (0, 0)))
    y = bias + xp[:, k_w - 1:k_w - 1 + t] * w[k_w - 1]
    for k in range(k_w - 1):
        y = y + xp[:, k:k + t] * w[k]
    return y


def chunked_gated_linear_attention(q, k, v, log_f):
    b_sz, t, h, dk = q.shape
    dv = v.shape[-1]
    n = t // CHUNK

    def to_chunks(a):
        return a.astype(jnp.float32).reshape(b_sz, n, CHUNK, h, a.shape[-1]).transpose(1, 0, 3, 2, 4)

    qc, kc, vc, gc = to_chunks(q), to_chunks(k), to_chunks(v), to_chunks(log_f)
    causal = jnp.tril(jnp.ones((CHUNK, CHUNK), dtype=bool))[:, :, None]

    def step(state, inp):
        qi, ki, vi, gi = inp
        cum = jnp.cumsum(gi, axis=2)
        diff = cum[:, :, :, None, :] - cum[:, :, None, :, :]
        decay = jnp.where(causal, jnp.exp(jnp.where(causal, diff, 0.0)), 0.0)
        scores = jnp.einsum('bhid,bhijd,bhjd->bhij', qi, decay, ki)
        last = cum[:, :, -1:, :]
        out = (jnp.einsum('bhij,bhjv->bhiv', scores, vi)
               + jnp.einsum('bhid,bhdv->bhiv', qi * jnp.exp(cum), state))
        state = (jnp.exp(last[:, :, 0, :, None]) * state
                 + jnp.einsum('bhjd,bhjv->bhdv', ki * jnp.exp(last - cum), vi))
        return state, out

    s0 = jnp.zeros((b_sz, h, dk, dv), jnp.float32)
    _, o = lax.scan(step, s0, (qc, kc, vc, gc))
    return o.transpose(1, 0, 3, 2, 4).reshape(b_sz, t, h, dv).astype(q.dtype)


def hgrn2_mixer(q, f_logit, i, g, lower_bound, norm_w):
    b_sz, t, _ = q.shape
    lb = lower_bound.astype(jnp.float32)
    z = f_logit.astype(jnp.float32)
    f = lb + (1.0 - lb) * jax.nn.sigmoid(z)
    log_f = jnp.log(jnp.maximum(f, F_FLOOR))
    key = (1.0 - lb) * jax.nn.sigmoid(-z)
    heads = lambda a, d: a.reshape(b_sz, t, A_HEADS, d)
    o = chunked_gated_linear_attention(heads(q, A_DK), heads(key, A_DK), heads(i, A_DV), heads(log_f, A_DK))
    return head_rmsnorm(o, norm_w) * jax.nn.silu(g)


def rglru_mixer(xb, gb, conv_w, conv_b, w_a, b_a, w_x, b_x, lam):
    b_sz, t, _ = xb.shape
    xc = causal_dwconv(xb, conv_w, conv_b)
    xh = xc.reshape(b_sz, t, B_BLOCKS, HEAD_DIM)
    r = jax.nn.sigmoid(jnp.einsum('bthi,hij->bthj', xh, w_a).reshape(b_sz, t, B_WIDTH) + b_a)
    gi = jax.nn.sigmoid(jnp.einsum('bthi,hij->bthj', xh, w_x).reshape(b_sz, t, B_WIDTH) + b_x)
    log_a = (-LRU_C * r.astype(jnp.float32)) * jax.nn.softplus(-lam.astype(jnp.float32))
    a = jnp.exp(log_a)
    u = jnp.sqrt(jnp.maximum(-jnp.expm1(2.0 * log_a), 0.0)) * (gi * xc).astype(jnp.float32)

    def combine(left, right):
        a1, u1 = left
        a2, u2 = right
        return a1 * a2, a2 * u1 + u2

    _, hs = lax.associative_scan(combine, (a, u), axis=1)
    return hs.astype(xb.dtype) * jax.nn.gelu(gb)


def gla_mixer(q, k, v, g, g_lr, w_g_up, b_g, norm_w):
    b_sz, t, _ = q.shape
    log_f = jax.nn.log_sigmoid((g_lr @ w_g_up + b_g).astype(jnp.float32)) / C_GATE_NORM
    qh = q.reshape(b_sz, t, C_HEADS, C_DK) * (C_DK ** -0.5)
    kh = k.reshape(b_sz, t, C_HEADS, C_DK)
    vh = v.reshape(b_sz, t, C_HEADS, C_DV)
    o = chunked_gated_linear_attention(qh, kh, vh, log_f.reshape(b_sz, t, C_HEADS, C_DK))
    return head_rmsnorm(o, norm_w) * jax.nn.silu(g)


def setup_inputs(seed: int = 0) -> dict:
    key = jax.random.key(seed)
    ks = jax.random.split(key, 24)
    f32 = jnp.float32

    def nrm(k, shape, scale):
        return scale * jax.random.normal(k, shape, f32)

    def gain(k, shape):
        return 1.0 + 0.05 * jax.random.normal(k, shape, f32)

    a0 = jax.random.uniform(ks[11], (DEPTH, B_WIDTH), f32, 0.9, 0.999)
    sig = a0 ** (1.0 / LRU_C)
    lam = jnp.log(sig) - jnp.log1p(-sig)
    return {
        'x': jax.random.normal(ks[0], (BATCH, SEQ, D_MODEL), f32),
        'norm_mix': gain(ks[1], (DEPTH, D_MODEL)),
        'w_in': nrm(ks[2], (DEPTH, D_MODEL, IN_COLS), D_MODEL ** -0.5),
        'hgrn_lower_bounds': nrm(ks[3], (DEPTH, A_HEADS * A_DK), 0.5),
        'hgrn_norm': gain(ks[4], (DEPTH, A_HEADS * A_DV)),
        'rg_conv_w': nrm(ks[5], (DEPTH, B_CONV, B_WIDTH), B_CONV ** -0.5),
        'rg_conv_b': nrm(ks[6], (DEPTH, B_WIDTH), 0.05),
        'rg_w_a': nrm(ks[7], (DEPTH, B_BLOCKS, HEAD_DIM, HEAD_DIM), HEAD_DIM ** -0.5),
        'rg_b_a': nrm(ks[8], (DEPTH, B_WIDTH), 0.1),
        'rg_w_x': nrm(ks[9], (DEPTH, B_BLOCKS, HEAD_DIM, HEAD_DIM), HEAD_DIM ** -0.5),
        'rg_b_x': nrm(ks[10], (DEPTH, B_WIDTH), 0.1),
        'rg_lambda': lam,
        'gla_w_gate_up': nrm(ks[12], (DEPTH, C_GATE_RANK, C_HEADS * C_DK), C_GATE_RANK ** -0.5),
        'gla_b_gate': nrm(ks[13], (DEPTH, C_HEADS * C_DK), 0.1),
        'gla_norm': gain(ks[14], (DEPTH, C_WIDTH)),
        'w_out': nrm(ks[15], (DEPTH, MIX_WIDTH, D_MODEL), MIX_WIDTH ** -0.5),
        'norm_ffn': gain(ks[16], (DEPTH, D_MODEL)),
        'ffn_w_gate': nrm(ks[17], (DEPTH, D_MODEL, D_FF), D_MODEL ** -0.5),
        'ffn_w_val': nrm(ks[18], (DEPTH, D_MODEL, D_FF), D_MODEL ** -0.5),
        'ffn_conv_w': nrm(ks[19], (DEPTH, FFN_CONV, D_FF), FFN_CONV ** -0.5),
        'ffn_conv_b': nrm(ks[20], (DEPTH, D_FF), 0.05),
        'ffn_w_down': nrm(ks[21], (DEPTH, D_FF, D_MODEL), D_FF ** -0.5),
        'norm_final': gain(ks[22], (D_MODEL,)),
    }


def reference(x, norm_mix, w_in, hgrn_lower_bounds, hgrn_norm, rg_conv_w, rg_conv_b, rg_w_a, rg_b_a,
              rg_w_x, rg_b_x, rg_lambda, gla_w_gate_up, gla_b_gate, gla_norm, w_out, norm_ffn,
              ffn_w_gate, ffn_w_val, ffn_conv_w, ffn_conv_b, ffn_w_down, norm_final):
    lb = jax.nn.softmax(hgrn_lower_bounds.astype(jnp.float32), axis=0)
    lb = jnp.cumsum(lb, axis=0) - lb[0]
    for l in range(DEPTH):
        h = rmsnorm(x, norm_mix[l])
        proj = h @ w_in[l]
        a_q, a_f, a_i, a_g, b_x, b_g, c_q, c_k, c_v, c_g, c_lr = jnp.split(proj, IN_SPLITS, axis=-1)
        o_a = hgrn2_mixer(a_q, a_f, a_i, a_g, lb[l], hgrn_norm[l])
        o_b = rglru_mixer(b_x, b_g, rg_conv_w[l], rg_conv_b[l], rg_w_a[l], rg_b_a[l],
                          rg_w_x[l], rg_b_x[l], rg_lambda[l])
        o_c = gla_mixer(c_q, c_k, c_v, c_g, c_lr, gla_w_gate_up[l], gla_b_gate[l], gla_norm[l])
        x = x + jnp.concatenate([o_a, o_b, o_c], axis=-1) @ w_out[l]
        h = rmsnorm(x, norm_ffn[l])
        gate = causal_dwconv(h @ ffn_w_gate[l], ffn_conv_w[l], ffn_conv_b[l])
        x = x + (jax.nn.gelu(gate) * (h @ ffn_w_val[l])) @ ffn_w_down[l]
    return rmsnorm(x, norm_final)
```

```python
import numpy as np
from contextlib import ExitStack
import concourse.bass as bass
import concourse.mybir as mybir
from concourse.bass_utils import run_bass_kernel_spmd

F32 = mybir.dt.float32
BF16 = mybir.dt.bfloat16
AF = mybir.ActivationFunctionType
ALU = mybir.AluOpType

D = 1024
TT = 512
NCI = 2832
DFF = 2688
NFF = 21
EPS = 1e-6
NSLOT = 4
NGRP = 28
DBG_T = 0
SLOT = 4096

_PL = {}
_off = 0
for _n, _c in [("norm_mix", 8), ("hgrn_norm", 2), ("cw0", 4), ("cw1", 4), ("cw2", 4), ("cw3", 4),
               ("conv_b", 4), ("b_a", 4), ("b_x", 4), ("lam", 4), ("gla_b", 1), ("gla_norm", 2),
               ("norm_ffn", 8), ("fw0", NFF), ("fw1", NFF), ("fw2", NFF), ("fb", NFF)]:
    _PL[_n] = (_off, _c)
    _off += _c
LP = _off
G_NF = 2 * LP
G_LB0 = G_NF + 8
G_LB1 = G_LB0 + 2
NP_COLS = G_LB1 + 2
C_ID = 0
C_M64 = 128
C_BDA = 192
C_BDC = 320
C_ONE = 576
NC_COLS = C_ONE + 512


def _fm(v):
    v = np.asarray(v, np.float32)
    return np.ascontiguousarray(v.reshape(-1, 128).T)


def _pack_params(inp):
    pp = np.zeros((128, NP_COLS), np.float32)
    for l in range(2):
        def put(name, v):
            o, c = _PL[name]
            pp[:, l * LP + o: l * LP + o + c] = _fm(v)
        put("norm_mix", inp["norm_mix"][l]); put("hgrn_norm", inp["hgrn_norm"][l])
        for k in range(4):
            put("cw%d" % k, inp["rg_conv_w"][l, k])
        put("conv_b", inp["rg_conv_b"][l]); put("b_a", inp["rg_b_a"][l]); put("b_x", inp["rg_b_x"][l])
        put("lam", inp["rg_lambda"][l]); put("gla_b", inp["gla_b_gate"][l]); put("gla_norm", inp["gla_norm"][l])
        put("norm_ffn", inp["norm_ffn"][l])
        for k in range(3):
            put("fw%d" % k, inp["ffn_conv_w"][l, k])
        put("fb", inp["ffn_conv_b"][l])
    pp[:, G_NF:G_NF + 8] = _fm(inp["norm_final"])
    pp[:, G_LB0:G_LB0 + 2] = _fm(inp["hgrn_lower_bounds"][0])
    pp[:, G_LB1:G_LB1 + 2] = _fm(inp["hgrn_lower_bounds"][1])
    return pp


def _const_mat():
    cm = np.zeros((128, NC_COLS), np.float32)
    p = np.arange(128)[:, None]
    cm[:, C_ID:C_ID + 128] = (p == np.arange(128)[None, :])
    cm[:, C_M64:C_M64 + 64] = ((p % 64) <= np.arange(64)[None, :])
    cm[:, C_BDA:C_BDA + 128] = ((p // 64) == (np.arange(128)[None, :] // 64))
    cm[:, C_BDC:C_BDC + 256] = ((p // 32) == (np.arange(256)[None, :] // 64))
    cm[:, C_ONE:C_ONE + 512] = 1.0
    return cm


class _Op:
    __slots__ = ("eng", "fn", "deps", "dma", "signal", "sem", "val", "n")


class Sched:
    def __init__(self):
        self.ops = []
        self.lastw = {}
        self.readers = {}

    cap = None

    def begin(self):
        self.cap = []

    def end(self):
        c = self.cap
        self.cap = None
        return c

    def merge(self, A, B):
        i = j = 0
        while i < len(A) or j < len(B):
            fa = i / max(len(A), 1)
            fb = j / max(len(B), 1)
            if j >= len(B) or (i < len(A) and fa <= fb):
                self.add(*A[i]); i += 1
            else:
                self.add(*B[j]); j += 1

    def add(self, eng, fn, reads=(), writes=(), dma=None):
        if self.cap is not None:
            self.cap.append((eng, fn, list(reads), list(writes), dma))
            return None
        op = _Op()
        op.eng = eng; op.fn = fn; op.dma = dma; op.signal = False; op.sem = None; op.val = 0
        deps = set()
        for k in reads:
            w = self.lastw.get(k)
            if w is not None:
                deps.add(w)
        for k in writes:
            w = self.lastw.get(k)
            if w is not None:
                deps.add(w)
            for r in self.readers.get(k, ()):
                deps.add(r)
        op.deps = [d for d in deps if not (d.dma is None and op.dma is None and d.eng == "pe" and eng == "pe")]
        for k in reads:
            self.readers.setdefault(k, []).append(op)
        for k in writes:
            self.lastw[k] = op
            self.readers[k] = []
        self.ops.append(op)
        return op

    def emit(self, nc, es):
        for op in self.ops:
            for d in op.deps:
                d.signal = True
        cnt = {}
        for op in self.ops:
            if not op.signal:
                continue
            if op.dma is not None:
                name = "d_" + op.dma
                cnt[name] = cnt.get(name, 0) + 16
            else:
                name = "e_" + op.eng
                cnt[name] = cnt.get(name, 0) + 1
            op.sem = name
            op.val = cnt[name]
        for nm in ("d_const",):
            if nm in cnt:
                for op in self.ops:
                    if op.sem == nm:
                        op.val = cnt[nm]
        sems = {name: es.enter_context(nc.semaphore(name)) for name in cnt}
        block = es.enter_context(nc.Block())
        engs = {"pe": block.tensor, "act": block.scalar, "dve": block.vector, "pool": block.gpsimd, "sp": block.sync}
        for ename, deco in engs.items():
            mine = [op for op in self.ops if op.eng == ename]

            def body(e, mine=mine):
                waited = {}
                for op in mine:
                    need = {}
                    for d in op.deps:
                        if d.val > need.get(d.sem, 0):
                            need[d.sem] = d.val
                    for s, v in need.items():
                        if v > waited.get(s, 0):
                            e.wait_ge(sems[s], v)
                            waited[s] = v
                    if op.fn is None:
                        continue
                    ins = op.fn(e)
                    if op.signal:
                        ins.then_inc(sems[op.sem], 16 if op.dma is not None else 1)
            deco(body)


def build(T, dbg=0):
    NT = T // TT
    nc = bass.Bass("TRN2", target_bir_lowering=False)
    x_d = nc.dram_tensor("x", [T, D], F32, kind="ExternalInput").ap()
    out_d = nc.dram_tensor("out", [T, D], F32, kind="ExternalOutput").ap()
    wall_d = nc.dram_tensor("wall", [2 * NGRP, 128, SLOT], F32, kind="ExternalInput").ap()
    pp_d = nc.dram_tensor("pp", [128, NP_COLS], F32, kind="ExternalInput").ap()
    cm_d = nc.dram_tensor("cm", [128, NC_COLS], F32, kind="ExternalInput").ap()
    rgw_d = nc.dram_tensor("rgw", [2, 2, 8, 64, 64], F32, kind="ExternalInput").ap()
    wgup_d = nc.dram_tensor("wgup", [2, 16, 128], F32, kind="ExternalInput").ap()

    S = Sched()
    es = ExitStack()
    dbg_d = nc.dram_tensor("dbg", [max(dbg, 1), 128, TT], F32, kind="ExternalOutput").ap() if dbg else None
    dbg_state = {"on": False}

    def tap(idx, ap, keys):
        if dbg and dbg_state["on"]:
            S.add("pool", lambda e: e.dma_start(out=dbg_d[idx], in_=ap), reads=keys, writes=[K("dbgd", idx)], dma="dbg")

    def sb(name, shape, dt):
        return es.enter_context(nc.sbuf_tensor("s_" + name, shape, dt))

    pp = sb("pp", [128, NP_COLS], F32)
    dv = sb("dv", [128, 64], F32)
    cm = sb("cm", [128, NC_COLS], F32)
    ident_bf = sb("ident_bf", [128, 128], BF16)
    ones_bf = sb("ones_bf", [128, 128], BF16)
    bda_bf = sb("bda_bf", [128, 128], BF16)
    wbd = sb("wbd", [128, 16, 128], BF16)
    wgup = sb("wgup", [16, 2, 128], F32)
    xin = [sb("xin%d" % i, [128, D], F32) for i in range(2)]
    xout = [sb("xout%d" % i, [128, D], F32) for i in range(2)]
    xT = sb("xT", [128, 8, TT], F32)
    hT = sb("hT", [128, 8, TT], BF16)
    ybuf = sb("ybuf", [128, NFF * TT], BF16)
    ybuf_f = ybuf.bitcast(F32)
    NSC = 10
    NSCB = 8
    NSCD = 4
    sc_all = sb("sc_all", [128, (NSC + NSCB + NSCD) * TT], F32)
    sc = [sc_all[:, i * TT:(i + 1) * TT] for i in range(NSC + NSCB + NSCD)]
    SB = sb("SB", [128, 8 * 128], BF16)
    explast0 = sb("explast0", [128, 8], F32)
    ngx = sb("ngx", [128, TT + 4], F32)
    bx = [[sb("bx%d_%d" % (l, j), [128, TT + 4], F32) for j in range(4)] for l in range(2)]
    hcar = sb("hcar", [128, 8], F32)
    qT = sb("qT", [128, TT], BF16)
    kT = sb("kT", [128, TT], BF16)
    khT = sb("khT", [128, TT], BF16)
    xcb = sb("xcb", [128, TT], BF16)
    osq = sb("osq", [128, TT], BF16)
    clr = sb("clr", [16, TT], F32)
    explast = sb("explast", [128, 8], F32)
    v_tm = sb("v_tm", [128, 4 * 256], BF16)
    vc_tm = sb("vc_tm", [128, 4 * 256], BF16)
    khat = sb("khat", [128, 4 * 128], BF16)
    sT = [sb("sT%d" % i, [128, 4 * 128], BF16) for i in range(4)]
    S32 = [[[sb("S32_%d_%d_%d" % (l, u, v), [128, 256 if u == 2 else 128], F32) for v in range(1)] for u in range(3)] for l in range(2)]
    Sbf = [[[sb("Sbf_%d_%d_%d" % (l, u, v), [128, 256 if u == 2 else 128], BF16) for v in range(1)]
            for u in range(3)] for l in range(2)]
    oT = sb("oT", [128, 8, TT], BF16)
    gbuf = sb("gbuf", [128, 4, TT + 2], F32)
    ghalo = sb("ghalo", [128, 2, NFF + 3, 2], F32)
    wbd_f = gbuf[:].rearrange("p a b -> p (a b)")[:, 0:2048].rearrange("p (a b) -> p a b", b=128)
    wslot = [sb("wslot%d" % i, [128, SLOT], BF16) for i in range(NSLOT)]
    banks = [es.enter_context(nc.psum_tensor("bank%d" % i, [128, TT], F32)) for i in range(8)]

    def K(*a):
        return tuple(a)

    YK = [K("y", c) for c in range(NFF)]

    def gate_view(g):
        return ybuf_f[:, g * TT:(g + 1) * TT], [YK[2 * g], YK[2 * g + 1]]

    def sq_view(c):
        return ybuf[:, c * TT:(c + 1) * TT], [YK[c]]

    def P(name, l, j=0):
        o, c = _PL[name]
        return pp[:, l * LP + o + j: l * LP + o + j + 1]

    DV_LB = 0
    DV_L1M = 4
    DV_M8 = 8
    DV_M16 = 16
    DV_NGB = 24
    DV_T = 32

    S.add("sp", lambda e: e.dma_start(out=pp[:], in_=pp_d), writes=[K("pp")], dma="const")
    S.add("sp", lambda e: e.dma_start(out=cm[:], in_=cm_d), writes=[K("cm")], dma="const")
    S.add("sp", lambda e: e.dma_start(out=wgup[:], in_=wgup_d.rearrange("l k n -> k l n")), writes=[K("wgup")], dma="const")
    S.add("dve", lambda e: e.memset(wbd_f, 0.0), writes=[K("wbd_f", i) for i in range(32)] + [K("gbuf", 0), K("gbuf", 1), K("gbufh", 0), K("gbufh", 1)])
    for l in range(2):
        for ax in range(2):
            for blk in range(8):
                j, hh = blk // 2, blk % 2
                idx = (l * 2 + ax) * 4 + j
                S.add("sp", lambda e, l=l, ax=ax, blk=blk, idx=idx, hh=hh: e.dma_start(
                    out=wbd_f[hh * 64:(hh + 1) * 64, idx, hh * 64:(hh + 1) * 64], in_=rgw_d[l, ax, blk]),
                    reads=[], writes=[K("wbd_f", (l * 2 + ax) * 8 + blk)], dma="const")
    S.add("dve", lambda e: e.tensor_copy(out=wbd[:], in_=wbd_f), reads=[K("wbd_f", i) for i in range(32)] + [K("gbuf", 0), K("gbuf", 1), K("gbufh", 0), K("gbufh", 1)], writes=[K("wbd")])
    S.add("dve", lambda e: e.tensor_copy(out=ident_bf[:], in_=cm[:, C_ID:C_ID + 128]), reads=[K("cm")], writes=[K("ident_bf")])
    S.add("dve", lambda e: e.tensor_copy(out=ones_bf[:], in_=cm[:, C_ONE:C_ONE + 128]), reads=[K("cm")], writes=[K("ones_bf")])
    S.add("dve", lambda e: e.tensor_copy(out=bda_bf[:], in_=cm[:, C_BDA:C_BDA + 128]), reads=[K("cm")], writes=[K("bda_bf")])
    ident = cm[:, C_ID:C_ID + 128]
    mask64 = cm[:, C_M64:C_M64 + 64]
    bdA = cm[:, C_BDA:C_BDA + 128]
    bdC = cm[:, C_BDC:C_BDC + 256]
    ones_f = cm[:, C_ONE:C_ONE + TT]
    S.add("dve", lambda e: e.memset(dv[:], 0.0), writes=[K("dv")])
    S.add("dve", lambda e: e.tensor_tensor(out=dv[:, DV_T:DV_T + 2], in0=pp[:, G_LB1:G_LB1 + 2], in1=pp[:, G_LB0:G_LB0 + 2], op=ALU.subtract),
          reads=[K("pp"), K("dv")], writes=[K("dvt")])
    S.add("act", lambda e: e.activation(out=dv[:, DV_LB + 2:DV_LB + 4], in_=dv[:, DV_T:DV_T + 2], func=AF.Sigmoid),
          reads=[K("dvt"), K("dv")], writes=[K("dvlb")])
    S.add("act", lambda e: e.activation(out=dv[:, DV_L1M:DV_L1M + 4], in_=dv[:, DV_LB:DV_LB + 4], func=AF.Ln, scale=-1.0, bias=1.0),
          reads=[K("dvlb")], writes=[K("dvl1m")])
    for l in range(2):
        o = _PL["lam"][0] + l * LP
        S.add("act", lambda e, o=o, l=l: e.activation(out=dv[:, DV_T + 4 + 4 * l:DV_T + 8 + 4 * l], in_=pp[:, o:o + 4], func=AF.Exp, scale=-1.0),
              reads=[K("pp"), K("dv")], writes=[K("dvt2", l)])
        S.add("act", lambda e, l=l: e.activation(out=dv[:, DV_T + 12 + 4 * l:DV_T + 16 + 4 * l], in_=dv[:, DV_T + 4 + 4 * l:DV_T + 8 + 4 * l], func=AF.Ln, bias=1.0),
              reads=[K("dvt2", l)], writes=[K("dvt3", l)])
        S.add("dve", lambda e, l=l: e.tensor_scalar(out=dv[:, DV_M8 + 4 * l:DV_M8 + 4 * l + 4], in0=dv[:, DV_T + 12 + 4 * l:DV_T + 16 + 4 * l], scalar1=-8.0, scalar2=None, op0=ALU.mult),
              reads=[K("dvt3", l)], writes=[K("dvm8", l)])
        S.add("dve", lambda e, l=l: e.tensor_scalar(out=dv[:, DV_M16 + 4 * l:DV_M16 + 4 * l + 4], in0=dv[:, DV_T + 12 + 4 * l:DV_T + 16 + 4 * l], scalar1=-16.0, scalar2=None, op0=ALU.mult),
              reads=[K("dvt3", l)], writes=[K("dvm16", l)])
        og = _PL["gla_b"][0] + l * LP
        S.add("dve", lambda e, l=l, og=og: e.tensor_scalar(out=dv[:, DV_NGB + l:DV_NGB + l + 1], in0=pp[:, og:og + 1], scalar1=-1.0, scalar2=None, op0=ALU.mult),
              reads=[K("pp"), K("dv")], writes=[K("dvngb", l)])
    DVK = [K("dvlb"), K("dvl1m")] + [K(n, l) for n in ("dvm8", "dvm16", "dvngb") for l in range(2)]
    for l in range(2):
        for u in range(3):
            S.add("dve", lambda e, l=l, u=u: e.memset(S32[l][u][0][:], 0.0), writes=[K("S32", l, u, 0), K("S32", l, u, 1)])
            S.add("dve", lambda e, l=l, u=u: e.memset(Sbf[l][u][0][:], 0.0), writes=[K("Sbf", l, u, 0), K("Sbf", l, u, 1)])
        for j in range(4):
            S.add("dve", lambda e, l=l, j=j: e.memset(bx[l][j][:], 0.0), writes=[K("bx", l, j)])
    S.add("dve", lambda e: e.memset(hcar[:], 0.0), writes=[K("hcar", 0), K("hcar", 1)])
    S.add("dve", lambda e: e.memset(ghalo[:], 0.0), writes=[K("ghalo", l_, g_, h_) for l_ in range(2) for g_ in range(6) for h_ in range(2)])
    S.add("dve", lambda e: e.memset(ngx[:], 0.0), writes=[K("ngx")])
    for i in range(4):
        S.add("dve", lambda e, i=i: e.memset(sT[i][:], 0.0), writes=[K("sT", i)])

    def layer_groups(l):
        gs = []
        for g in range(6):
            c0 = g * 512
            n = min(512, NCI - c0)
            gs.append(("in", l, c0, n))
        for g in range(2):
            gs.append(("out", l, g * 512, 512))
        for g in range(6):
            c0 = g * 512
            n = min(512, DFF - c0)
            gs.append(("gate", l, c0, n))
            gs.append(("val", l, c0, n))
        for dt in range(8):
            gs.append(("down", l, dt * 128, 128))
        return gs

    stream = []
    for t in range(NT):
        for l in range(2):
            stream += layer_groups(l)
    wstate = {"next_load": 0, "next_use": 0}

    def issue_load(i):
        kind, l, c0, n = stream[i]
        s = i % NSLOT
        gi = l * NGRP + (i % (2 * NGRP)) % NGRP
        kk = NFF if kind == "down" else 8
        src = wall_d[gi, :, 0:kk * n]
        dst = wslot[s][:, 0:kk * n]
        S.add("pool", lambda e, src=src, dst=dst: e.dma_start(out=dst, in_=src), writes=[K("wslot", s)], dma="w%d" % s)

    def next_weights(expect):
        i = wstate["next_use"]
        while wstate["next_load"] <= min(len(stream) - 1, i + NSLOT - 2):
            issue_load(wstate["next_load"])
            wstate["next_load"] += 1
        kind, l, c0, n = stream[i]
        assert kind == expect[0] and l == expect[1], (stream[i], expect)
        s = i % NSLOT
        kk = NFF if kind == "down" else 8
        view = wslot[s][:, 0:kk * n].rearrange("p (k n) -> p k n", k=kk)
        wstate["next_use"] += 1
        return view, K("wslot", s), n

    rot = {"proj": 0, "psu": 0, "ffn": 0, "mod": 3}

    def bank_proj():
        b = rot["proj"] % rot["mod"]
        rot["proj"] += 1
        return banks[b], K("bank", b)

    def bank_psu():
        b = 6 + rot["psu"] % 2
        rot["psu"] += 1
        return banks[b], K("bank", b)

    def bank_ffn():
        b = rot["ffn"] % 8
        rot["ffn"] += 1
        return banks[b], K("bank", b)

    sci = {"i": 0}

    def scratch():
        i = sci["i"] % NSC
        sci["i"] += 1
        return sc[i], K("sc", i)

    scbi = {"i": 0}

    def scratchB():
        i = NSC + scbi["i"] % NSCB
        scbi["i"] += 1
        return sc[i], K("sc", i)

    def scratch2():
        if sci["i"] % 2:
            sci["i"] += 1
        i = sci["i"] % NSC
        sci["i"] += 2
        return sc_all[:, i * TT:(i + 2) * TT], [K("sc", i), K("sc", i + 1)]

    def act(out, in_, func, reads, writes, bias=None, scale=None):
        kw = {}
        if bias is not None:
            kw["bias"] = bias
        if scale is not None:
            kw["scale"] = scale
        S.add("act", lambda e: e.activation(out=out, in_=in_, func=func, **kw), reads=reads, writes=writes)

    def tt(out, in0, in1, op, reads, writes, eng="dve"):
        S.add(eng, lambda e: e.tensor_tensor(out=out, in0=in0, in1=in1, op=op), reads=reads, writes=writes)

    def ts(out, in0, s1, s2, op0, op1, reads, writes, eng="dve"):
        if s2 is None:
            S.add(eng, lambda e: e.tensor_scalar(out=out, in0=in0, scalar1=s1, scalar2=None, op0=op0), reads=reads, writes=writes)
        else:
            S.add(eng, lambda e: e.tensor_scalar(out=out, in0=in0, scalar1=s1, scalar2=s2, op0=op0, op1=op1), reads=reads, writes=writes)

    def stt(out, in0, scalar, in1, op0, op1, reads, writes, eng="dve"):
        S.add(eng, lambda e: e.scalar_tensor_tensor(out=out, in0=in0, scalar=scalar, in1=in1, op0=op0, op1=op1), reads=reads, writes=writes)

    def mm_group(mms, reads, writes):
        def fn(e):
            ins = None
            for m in mms:
                kw = dict(start=m["start"], stop=m["stop"])
                if m.get("tp") is not None:
                    kw["tile_position"] = m["tp"]
                if m.get("skip"):
                    kw["skip_group_check"] = True
                ins = e.matmul(m["out"], m["lhsT"], m["rhs"], **kw)
            return ins
        S.add("pe", fn, reads=reads, writes=writes)

    def proj_fm(wv, wk, col0, m, reads_h, bank, bkey):
        mms = [dict(out=bank[0:m, :], lhsT=wv[:, kc, col0:col0 + m], rhs=hT[:, kc, :], start=(kc == 0), stop=(kc == 7)) for kc in range(8)]
        mm_group(mms, reads=[wk] + reads_h, writes=[bkey])

    HK = [K("hT")]

    def rmsnorm(gcol, out_bf16=True, final=False):
        bank, bkey = bank_proj()
        for c in range(8):
            sqv, sqk = sq_view(c)
            act(sqv, xT[:, c, :], AF.Square, reads=[K("xT", c)], writes=sqk)
        mms = []
        rk = []
        for c in range(8):
            sqv, sqk = sq_view(c)
            mms.append(dict(out=bank[:, :], lhsT=ones_bf[:], rhs=sqv, start=(c == 0), stop=(c == 7)))
            rk += sqk
        mm_group(mms, reads=rk + [K("ones_bf")], writes=[bkey])
        lnv, lk = scratch()
        act(lnv[:], bank[:, :], AF.Ln, reads=[bkey], writes=[lk], scale=1.0 / D, bias=EPS)
        rs, rsk = scratch()
        act(rs[:], lnv[:], AF.Exp, reads=[lk], writes=[rsk], scale=-0.5)
        outs = []
        for c in range(8):
            if final:
                o_, ok = scratch()
                stt(o_[:], xT[:, c, :], gcol(c), rs[:], ALU.mult, ALU.mult, reads=[K("xT", c), rsk, K("pp")], writes=[ok])
                outs.append((o_, ok))
            else:
                stt(hT[:, c, :], xT[:, c, :], gcol(c), rs[:], ALU.mult, ALU.mult, reads=[K("xT", c), rsk, K("pp")], writes=HK)
        return outs

    def cumdecay(nlf_ap, nlfk):
        S.add("dve", lambda e: e.tensor_tensor_scan(out=ngx[:, 1:TT + 1], data0=ones_f, data1=nlf_ap, initial=0.0, op0=ALU.mult, op1=ALU.add),
              reads=[nlfk, K("cm")], writes=[K("ngx")])
        cl, clk = scratch()
        cl3 = cl[:].rearrange("p (c i) -> p c i", i=64)
        n3 = ngx[:, 1:TT + 1].rearrange("p (c i) -> p c i", i=64)
        b3 = ngx[:, 0:TT].rearrange("p (c i) -> p c i", i=64)[:, :, 0:1].to_broadcast([128, 8, 64])
        tt(cl3, n3, b3, ALU.subtract, reads=[K("ngx")], writes=[clk])
        return cl, clk

    def khat_transposes(dstkey):
        bank, bkey = bank_proj()
        bb = bank.bitcast(BF16)
        def fn(e):
            ins = None
            for blk in range(4):
                ins = e.transpose(out=bb[:, blk * 128:(blk + 1) * 128], in_=khT[:, blk * 128:(blk + 1) * 128], identity=ident_bf[:])
            return ins
        S.add("pe", fn, reads=[K("khT"), K("ident_bf")], writes=[bkey])
        act(khat[:], bb[:, 0:512], AF.Copy, reads=[bkey], writes=[dstkey])

    def gla_unit(l, u, vbuf, vkey, vcol0, nv, heads, pairs, bdm):
        for hi, (rb, dk, vc, pi, hh) in enumerate(heads):
            sb_i = 3
            bank, bkey = banks[sb_i], K("bank", sb_i)
            mms = []
            for blk in range(4):
                mms.append(dict(out=bank[:, blk * 128:(blk + 1) * 128], lhsT=kT[rb:rb + dk, blk * 128:(blk + 1) * 128],
                                rhs=qT[rb:rb + dk, blk * 128:(blk + 1) * 128], start=True, stop=True,
                                tp=(rb, 0) if rb == 96 else None))
            mm_group(mms, reads=[K("kT"), K("qT")], writes=[bkey])
            b3 = bank[:, :].rearrange("p (b i) -> p b i", i=128)
            s3 = sT[hi][:].rearrange("p (b i) -> p b i", i=128)
            for q in range(2):
                tt(s3[q * 64:(q + 1) * 64, :, q * 64:(q + 1) * 64], b3[q * 64:(q + 1) * 64, :, q * 64:(q + 1) * 64],
                   mask64[q * 64:(q + 1) * 64, :].unsqueeze(1).to_broadcast([64, 4, 64]), ALU.mult,
                   reads=[bkey, K("cm")], writes=[K("sT", hi)])
        if u == 0:
            tap(29, sT[0][:], [K("sT", 0)]); tap(30, sT[1][:], [K("sT", 1)])
        pob = [(banks[4 + pi], K("bank", 4 + pi)) for pi in range(len(pairs))]
        for pi in range(len(pairs)):
            po, pok = pob[pi]
            mms = []
            rk = [vkey]
            for hi, (rb, dk, vc, hpi, hh) in enumerate(heads):
                if hpi != pi:
                    continue
                rk.append(K("sT", hi))
                for blk in range(4):
                    mms.append(dict(out=po[hh * 64:(hh + 1) * 64, blk * 128:(blk + 1) * 128],
                                    lhsT=vbuf[:, blk * 256 + vc: blk * 256 + vc + 64],
                                    rhs=sT[hi][:, blk * 128:(blk + 1) * 128], start=(blk == 0), stop=False, skip=True, tp=(0, 64) if hh == 1 else None))
            mm_group(mms, reads=rk, writes=[pok])
        ex0 = explast0
        S.add("dve", lambda e: e.tensor_copy(out=ex0[:], in_=explast[:]), reads=[K("explast")], writes=[K("ex0")])
        S.add("dve", lambda e: e.memset(ex0[:, 0:1], 0.0), reads=[K("ex0")], writes=[K("ex0")])
        E8, E8k = scratch2()
        E83 = E8.rearrange("p (v c) -> p v c", c=8)
        S.add("dve", lambda e: e.tensor_copy(out=E83, in_=ex0[:].unsqueeze(1).to_broadcast([128, 128, 8])), reads=[K("ex0")], writes=E8k)
        for g in range(len(pairs)):
            po, pok = pob[g]
            vc0 = vcol0 + g * 128
            s32 = S32[l][u][0][:, g * 128:(g + 1) * 128]
            sbf0 = Sbf[l][u][0][:, g * 128:(g + 1) * 128]
            s32k = K("S32", l, u, g)
            sbfk = K("Sbf", l, u, g)
            for b in range(2):
                mms = []
                for cc in range(4):
                    c = 2 * cc + b
                    r0 = (c % 2) * 64
                    blk = c // 2
                    mms.append(dict(out=banks[6 + b][:, cc * 128:(cc + 1) * 128], lhsT=khat[r0:r0 + 64, blk * 128:(blk + 1) * 128],
                                    rhs=vbuf[r0:r0 + 64, blk * 256 + vc0: blk * 256 + vc0 + 128], start=True, stop=True))
                mm_group(mms, reads=[K("khat"), vkey], writes=[K("bank", 6 + b)])
            PUM, PUMk = scratch2()
            PUM3 = PUM.rearrange("p (v c) -> p v c", c=8)
            PUM4 = PUM.rearrange("p (v cc two) -> p v cc two", cc=4, two=2)
            mk = bdm[:, g * 128:(g + 1) * 128]
            for b in range(2):
                src = banks[6 + b][:, :].rearrange("p (c v) -> p v c", v=128)
                tt(PUM4[:, :, :, b], src, mk.unsqueeze(2).to_broadcast([128, 128, 4]), ALU.mult,
                   reads=[K("bank", 6 + b), K("cm")], writes=PUMk)
            stt(PUM3[:, :, 0], s32, explast[:, 0:1], PUM3[:, :, 0], ALU.mult, ALU.add, reads=[s32k, K("explast")] + PUMk, writes=PUMk)
            ST, STk = scratch2()
            S.add("dve", lambda e, ST=ST, E8=E8, PUM=PUM: e.tensor_tensor_scan(out=ST, data0=E8, data1=PUM, initial=0.0, op0=ALU.mult, op1=ALU.add),
                  reads=E8k + PUMk, writes=STk)
            ST3 = ST.rearrange("p (v c) -> p v c", c=8)
            mm_group([dict(out=po[:, 0:64], lhsT=sbf0, rhs=qT[:, 0:64], start=False, stop=True, skip=True)],
                     reads=[sbfk, K("qT"), pok], writes=[pok])
            S.add("dve", lambda e, ST3=ST3: e.tensor_copy(out=SB[:].rearrange("p (c v) -> p v c", v=128), in_=ST3), reads=STk, writes=[K("SB")])
            S.add("dve", lambda e, s32=s32, ST3=ST3: e.tensor_copy(out=s32, in_=ST3[:, :, 7]), reads=STk, writes=[s32k])
            mms = []
            for c in range(1, 8):
                mms.append(dict(out=po[:, c * 64:(c + 1) * 64], lhsT=SB[:, (c - 1) * 128:c * 128], rhs=qT[:, c * 64:(c + 1) * 64],
                                start=False, stop=True, skip=True))
            mm_group(mms, reads=[K("SB"), K("qT"), pok], writes=[pok])
            S.add("dve", lambda e, sbf0=sbf0: e.tensor_copy(out=sbf0, in_=SB[:, 7 * 128:8 * 128]), reads=[K("SB")], writes=[sbfk])
        for pi, (ochunk, gate_ap, gate_keys, normcol) in enumerate(pairs):
            po, pok = pob[pi]
            act(osq[:], po[:, :], AF.Square, reads=[pok], writes=[K("osq")])
            pn, pnk = bank_proj()
            mm_group([dict(out=pn[:, :], lhsT=bda_bf[:], rhs=osq[:], start=True, stop=True)], reads=[K("osq"), K("bda_bf")], writes=[pnk])
            lnv, lk = scratch()
            act(lnv[:], pn[:, :], AF.Ln, reads=[pnk], writes=[lk], scale=1.0 / 64, bias=EPS)
            rs, rsk = scratch()
            act(rs[:], lnv[:], AF.Exp, reads=[lk], writes=[rsk], scale=-0.5)
            on, onk = scratch()
            tt(on[:], po[:, :], rs[:], ALU.mult, reads=[pok, rsk], writes=[onk])
            if u == 0:
                tap(31, on[:], [onk]); tap(32, rs[:], [rsk])
            stt(oT[:, ochunk, :], on[:], normcol, gate_ap, ALU.mult, ALU.mult, reads=[onk, K("pp")] + gate_keys, writes=[K("oT", ochunk)])

    xblk = {"i": 0}
    oblk = {"i": 0}

    def load_tile(t):
        staged = []
        for blk in range(4):
            i = xblk["i"] % 2
            xblk["i"] += 1
            r0 = t * TT + blk * 128
            S.add("sp", lambda e, i=i, r0=r0: e.dma_start(out=xin[i][:], in_=x_d[r0:r0 + 128, :]), writes=[K("xin", i)], dma="xin%d" % i)
            staged.append(i)
            for half in range(2):
                bank, bkey = bank_ffn()
                def fn(e, i=i, half=half, bank=bank):
                    ins = None
                    for cc in range(4):
                        c = half * 4 + cc
                        ins = e.transpose(out=bank[:, cc * 128:(cc + 1) * 128], in_=xin[i][:, c * 128:(c + 1) * 128], identity=ident)
                    return ins
                S.add("pe", fn, reads=[K("xin", i), K("cm")], writes=[bkey])
                dst = xT[:, half * 4:(half + 1) * 4, blk * 128:(blk + 1) * 128]
                src = bank[:, :].rearrange("p (c i) -> p c i", i=128)
                S.add("act" if half == 0 else "dve",
                      (lambda e, dst=dst, src=src: e.activation(out=dst, in_=src, func=AF.Copy)) if half == 0 else
                      (lambda e, dst=dst, src=src: e.tensor_copy(out=dst, in_=src)),
                      reads=[bkey], writes=[K("xT", c) for c in range(half * 4, half * 4 + 4)])

    def store_tile(t, outs):
        for blk in range(4):
            i = oblk["i"] % 2
            oblk["i"] += 1
            for half in range(2):
                bank, bkey = bank_ffn()
                def fn(e, half=half, bank=bank, blk=blk):
                    ins = None
                    for cc in range(4):
                        c = half * 4 + cc
                        ins = e.transpose(out=bank[:, cc * 128:(cc + 1) * 128], in_=outs[c][0][:, blk * 128:(blk + 1) * 128], identity=ident)
                    return ins
                S.add("pe", fn, reads=[outs[c][1] for c in range(half * 4, half * 4 + 4)] + [K("cm")], writes=[bkey])
                dst = xout[i][:, half * 512:(half + 1) * 512]
                if half == 0:
                    S.add("act", lambda e, dst=dst, bank=bank: e.activation(out=dst, in_=bank[:, :], func=AF.Copy), reads=[bkey], writes=[K("xout", i)])
                else:
                    S.add("dve", lambda e, dst=dst, bank=bank: e.tensor_copy(out=dst, in_=bank[:, :]), reads=[bkey], writes=[K("xout", i)])
            r0 = t * TT + blk * 128
            S.add("sp", lambda e, i=i, r0=r0: e.dma_start(out=out_d[r0:r0 + 128, :], in_=xout[i][:]), reads=[K("xout", i)], writes=[K("outd", r0)], dma="xout%d" % i)

    def mixer(l):
        rmsnorm(lambda c: P("norm_mix", l, c))
        wv0, wk0, _ = next_weights(("in", l))
        wv1, wk1, _ = next_weights(("in", l))
        for half in range(2):
            bank, bkey = bank_proj()
            mms = []
            for bb_ in range(2):
                blk = half * 2 + bb_
                for kc in range(8):
                    mms.append(dict(out=bank[:, bb_ * 256:(bb_ + 1) * 256], lhsT=hT[:, kc, blk * 128:(blk + 1) * 128],
                                    rhs=wv1[:, kc, 0:256], start=(kc == 0), stop=(kc == 7)))
            mm_group(mms, reads=[wk1] + HK, writes=[bkey])
            act(v_tm[:, half * 512:(half + 1) * 512], bank[:, :], AF.Copy, reads=[bkey], writes=[K("v_tm")])
        gateA = []
        for p in range(2):
            bank, bkey = bank_proj()
            proj_fm(wv1, wk1, 256 + p * 128, 128, HK, bank, bkey)
            gv, gk = gate_view(p)
            act(gv, bank[:, :], AF.Silu, reads=[bkey], writes=gk)
            gateA.append((gv, gk))
        aqz = []
        for p in range(2):
            bank, bkey = bank_proj()
            proj_fm(wv0, wk0, p * 128, 128, HK, bank, bkey)
            aq, aqk = sc[NSC + NSCB + 2 * p], K("sc", NSC + NSCB + 2 * p)
            act(aq[:], bank[:, :], AF.Copy, reads=[bkey], writes=[aqk])
            zb, zk_ = bank_proj()
            proj_fm(wv0, wk0, 256 + p * 128, 128, HK, zb, zk_)
            zs, zsk = sc[NSC + NSCB + 2 * p + 1], K("sc", NSC + NSCB + 2 * p + 1)
            act(zs[:], zb[:, :], AF.Copy, reads=[zk_], writes=[zsk])
            aqz.append((aq, aqk, zs, zsk))
        wv2, wk2, _ = next_weights(("in", l))
        for j in range(4):
            bank, bkey = bank_proj()
            proj_fm(wv2, wk2, j * 128, 128, HK, bank, bkey)
            ts(bx[l][j][:, 0:3], bx[l][j][:, TT:TT + 3], 1.0, None, ALU.mult, None, reads=[K("bx", l, j)], writes=[K("bxh", l, j)])
            act(bx[l][j][:, 3:TT + 3], bank[:, :], AF.Copy, reads=[bkey, K("bxh", l, j)], writes=[K("bx", l, j)])
        wv3, wk3, _ = next_weights(("in", l))
        gateB = []
        for j in range(4):
            bank, bkey = bank_proj()
            proj_fm(wv3, wk3, j * 128, 128, HK, bank, bkey)
            gv, gk = gate_view(2 + j)
            act(gv, bank[:, :], AF.Gelu_apprx_tanh, reads=[bkey], writes=gk)
            gateB.append((gv, gk))
        S.begin()
        for p in range(2):
            aq, aqk, zb, zk = aqz[p]
            E, Ek = scratch()
            act(E[:], zb[:], AF.Exp, reads=[zk], writes=[Ek], scale=-1.0)
            L1, L1k = scratch()
            act(L1[:], E[:], AF.Ln, reads=[Ek], writes=[L1k], bias=1.0)
            L2, L2k = scratch()
            act(L2[:], E[:], AF.Ln, reads=[Ek] + DVK, writes=[L2k], bias=1.0, scale=dv[:, DV_LB + 2 * l + p:DV_LB + 2 * l + p + 1])
            W, Wk = scratch()
            tt(W[:], zb[:], L1[:], ALU.add, reads=[zk, L1k], writes=[Wk], eng="pool")
            nlf, nlfk = scratch()
            tt(nlf[:], L1[:], L2[:], ALU.subtract, reads=[L1k, L2k], writes=[nlfk], eng="pool")
            cl, clk = cumdecay(nlf[:], nlfk)
            qe, qek = scratch()
            act(qe[:], cl[:], AF.Exp, reads=[clk], writes=[qek], scale=-1.0)
            tt(qT[:], aq[:], qe[:], ALU.mult, reads=[aqk, qek], writes=[K("qT")])
            ak, akk = scratch()
            tt(ak[:], cl[:], W[:], ALU.subtract, reads=[clk, Wk], writes=[akk], eng="pool")
            act(kT[:], ak[:], AF.Exp, reads=[akk] + DVK, writes=[K("kT")], bias=dv[:, DV_L1M + 2 * l + p:DV_L1M + 2 * l + p + 1])
            cl3 = cl[:].rearrange("p (c i) -> p c i", i=64)
            act(explast[:], cl3[:, :, 63], AF.Exp, reads=[clk], writes=[K("explast")], scale=-1.0)
            tt(khT[:].rearrange("p (c i) -> p c i", i=64), kT[:].rearrange("p (c i) -> p c i", i=64),
               explast[:].unsqueeze(2).to_broadcast([128, 8, 64]), ALU.mult, reads=[K("kT"), K("explast")], writes=[K("khT")])
            khat_transposes(K("khat"))
            if p == 0:
                tap(24, qT[:], [K("qT")]); tap(25, kT[:], [K("kT")]); tap(26, khat[:], [K("khat")]); tap(27, cl[:], [clk])
                tap(28, v_tm[:, 0:512], [K("v_tm")])
            heads = [(0, 64, p * 128, 0, 0), (64, 64, p * 128 + 64, 0, 1)]
            pairs = [(p, gateA[p][0], gateA[p][1], P("hgrn_norm", l, p))]
            gla_unit(l, p, v_tm, K("v_tm"), p * 128, 128, heads, pairs, bdA)
        capA = S.end()
        def rg_chunk(j, rgb):
            b_ = bx[l][j]
            bk = [K("bx", l, j), K("bxh", l, j), K("pp")]
            xc, xck = scratchB()
            ts(xc[:], b_[:, 3:TT + 3], P("cw3", l, j), P("conv_b", l, j), ALU.mult, ALU.add, reads=bk, writes=[xck])
            for k in range(3):
                stt(xc[:], b_[:, k:k + TT], P("cw%d" % k, l, j), xc[:], ALU.mult, ALU.add, reads=bk + [xck], writes=[xck])
            act(xcb[:], xc[:], AF.Copy, reads=[xck], writes=[K("xcb")])
            pr, prk = banks[rgb], K("bank", rgb)
            mm_group([dict(out=pr[:, :], lhsT=wbd[:, (l * 2 + 0) * 4 + j, :], rhs=xcb[:], start=True, stop=True)], reads=[K("xcb"), K("wbd")], writes=[prk])
            r, rk = scratchB()
            act(r[:], pr[:, :], AF.Sigmoid, reads=[prk, K("pp")], writes=[rk], bias=P("b_a", l, j))
            pg, pgk = banks[rgb], K("bank", rgb)
            mm_group([dict(out=pg[:, :], lhsT=wbd[:, (l * 2 + 1) * 4 + j, :], rhs=xcb[:], start=True, stop=True)], reads=[K("xcb"), K("wbd")], writes=[pgk])
            gi, gik = scratchB()
            act(gi[:], pg[:, :], AF.Sigmoid, reads=[pgk, K("pp")], writes=[gik], bias=P("b_x", l, j))
            a, ak_ = scratchB()
            act(a[:], r[:], AF.Exp, reads=[rk] + DVK, writes=[ak_], scale=dv[:, DV_M8 + 4 * l + j:DV_M8 + 4 * l + j + 1])
            a2, a2k = scratchB()
            act(a2[:], r[:], AF.Exp, reads=[rk] + DVK, writes=[a2k], scale=dv[:, DV_M16 + 4 * l + j:DV_M16 + 4 * l + j + 1])
            ts(a2[:], a2[:], 1.0, -1e-12, ALU.subtract, ALU.min, reads=[a2k], writes=[a2k])
            act(a2[:], a2[:], AF.Ln, reads=[a2k], writes=[a2k], scale=-1.0)
            act(a2[:], a2[:], AF.Exp, reads=[a2k], writes=[a2k], scale=0.5)
            tt(gi[:], gi[:], xc[:], ALU.mult, reads=[gik, xck], writes=[gik], eng="pool")
            tt(gi[:], gi[:], a2[:], ALU.mult, reads=[gik, a2k], writes=[gik], eng="pool")
            hh_, hk = scratchB()
            S.add("dve", lambda e, hh_=hh_, a=a, gi=gi, l=l, j=j: e.tensor_tensor_scan(
                out=hh_[:], data0=a[:], data1=gi[:], initial=hcar[:, l * 4 + j:l * 4 + j + 1], op0=ALU.mult, op1=ALU.add),
                reads=[ak_, gik, K("hcar", l)], writes=[hk])
            ts(hcar[:, l * 4 + j:l * 4 + j + 1], hh_[:, TT - 1:TT], 1.0, None, ALU.mult, None, reads=[hk], writes=[K("hcar", l)])
            tt(oT[:, 2 + j, :], hh_[:], gateB[j][0], ALU.mult, reads=[hk] + gateB[j][1], writes=[K("oT", 2 + j)])
        S.begin()
        rg_chunk(0, 5); rg_chunk(1, 5)
        capB1 = S.end()
        S.begin()
        rg_chunk(2, 2); rg_chunk(3, 2)
        capB2 = S.end()
        S.merge(capA, capB1)
        wv4, wk4, _ = next_weights(("in", l))
        for half in range(2):
            bank, bkey = bank_proj()
            mms = []
            for bb_ in range(2):
                blk = half * 2 + bb_
                for kc in range(8):
                    mms.append(dict(out=bank[:, bb_ * 256:(bb_ + 1) * 256], lhsT=hT[:, kc, blk * 128:(blk + 1) * 128],
                                    rhs=wv4[:, kc, 256:512], start=(kc == 0), stop=(kc == 7)))
            mm_group(mms, reads=[wk4] + HK, writes=[bkey])
            act(vc_tm[:, half * 512:(half + 1) * 512], bank[:, :], AF.Copy, reads=[bkey], writes=[K("vc_tm")])
        bank, bkey = bank_proj()
        proj_fm(wv4, wk4, 0, 128, HK, bank, bkey)
        cq, cqk = scratch()
        act(cq[:], bank[:, :], AF.Copy, reads=[bkey], writes=[cqk], scale=32.0 ** -0.5)
        bank, bkey = bank_proj()
        proj_fm(wv4, wk4, 128, 128, HK, bank, bkey)
        ck, ckk = scratch()
        act(ck[:], bank[:, :], AF.Copy, reads=[bkey], writes=[ckk])
        wv5, wk5, _ = next_weights(("in", l))
        gateC = []
        for p in range(2):
            bank, bkey = bank_proj()
            proj_fm(wv5, wk5, p * 128, 128, HK, bank, bkey)
            gv, gk = gate_view(6 + p)
            act(gv, bank[:, :], AF.Silu, reads=[bkey], writes=gk)
            gateC.append((gv, gk))
        bank, bkey = bank_proj()
        proj_fm(wv5, wk5, 256, 16, HK, bank, bkey)
        act(clr[:], bank[0:16, :], AF.Copy, reads=[bkey], writes=[K("clr")])
        S.begin()
        rot["mod"] = 2
        pgb, pgbk = bank_proj()
        mm_group([dict(out=pgb[:, :], lhsT=wgup[:, l, :], rhs=clr[:], start=True, stop=True)], reads=[K("clr"), K("wgup")], writes=[pgbk])
        E, Ek = scratch()
        act(E[:], pgb[:, :], AF.Exp, reads=[pgbk] + DVK, writes=[Ek], scale=-1.0, bias=dv[:, DV_NGB + l:DV_NGB + l + 1])
        L, Lk = scratch()
        act(L[:], E[:], AF.Ln, reads=[Ek], writes=[Lk], bias=1.0)
        cl, clk = cumdecay(L[:], Lk)
        qe, qek = scratch()
        act(qe[:], cl[:], AF.Exp, reads=[clk], writes=[qek], scale=-1.0 / 16)
        tt(qT[:], cq[:], qe[:], ALU.mult, reads=[cqk, qek], writes=[K("qT")])
        ke, kek = scratch()
        act(ke[:], cl[:], AF.Exp, reads=[clk], writes=[kek], scale=1.0 / 16)
        tt(kT[:], ck[:], ke[:], ALU.mult, reads=[ckk, kek], writes=[K("kT")])
        cl3 = cl[:].rearrange("p (c i) -> p c i", i=64)
        act(explast[:], cl3[:, :, 63], AF.Exp, reads=[clk], writes=[K("explast")], scale=-1.0 / 16)
        tt(khT[:].rearrange("p (c i) -> p c i", i=64), kT[:].rearrange("p (c i) -> p c i", i=64),
           explast[:].unsqueeze(2).to_broadcast([128, 8, 64]), ALU.mult, reads=[K("kT"), K("explast")], writes=[K("khT")])
        khat_transposes(K("khat"))
        heads = [(32 * h, 32, 64 * h, h // 2, h % 2) for h in range(4)]
        pairs = [(6 + p, gateC[p][0], gateC[p][1], P("gla_norm", l, p)) for p in range(2)]
        gla_unit(l, 2, vc_tm, K("vc_tm"), 0, 256, heads, pairs, bdC)
        rot["mod"] = 3
        capC = S.end()
        S.merge(capC, capB2)
        for g in range(2):
            wv, wk, _ = next_weights(("out", l))
            for dt in range(4):
                d = g * 4 + dt
                bank, bkey = bank_proj()
                mms = [dict(out=bank[:, :], lhsT=wv[:, kc, dt * 128:(dt + 1) * 128], rhs=oT[:, kc, :], start=(kc == 0), stop=(kc == 7)) for kc in range(8)]
                mm_group(mms, reads=[wk] + [K("oT", kc) for kc in range(8)], writes=[bkey])
                tt(xT[:, d, :], xT[:, d, :], bank[:, :], ALU.add, reads=[K("xT", d), bkey], writes=[K("xT", d)])

    def ffn(l):
        rmsnorm(lambda c: P("norm_ffn", l, c))
        for g in range(6):
            wvg, wkg, n = next_weights(("gate", l))
            wvv, wkv, _ = next_weights(("val", l))
            nch = n // 128
            c0 = g * 4
            halves = [list(range(h0, min(h0 + 2, nch))) for h0 in range(0, nch, 2)]
            for hf, ccs in enumerate(halves):
                lo, hi = ccs[0], ccs[-1] + 1
                S.add("dve", lambda e, lo=lo, hi=hi, c0=c0: e.tensor_copy(out=gbuf[:, lo:hi, 0:2], in_=ghalo[:, l, c0 + lo:c0 + hi, :]),
                      reads=[K("ghalo", l, g, hf), K("gbuf", hf)], writes=[K("gbufh", hf)])
                for cc in ccs:
                    bank, bkey = bank_ffn()
                    proj_fm(wvg, wkg, cc * 128, 128, HK, bank, bkey)
                    act(gbuf[:, cc, 2:TT + 2], bank[:, :], AF.Copy, reads=[bkey, K("gbufh", hf)], writes=[K("gbuf", hf)])
                S.add("dve", lambda e, lo=lo, hi=hi, c0=c0: e.tensor_copy(out=ghalo[:, l, c0 + lo:c0 + hi, :], in_=gbuf[:, lo:hi, TT:TT + 2]),
                      reads=[K("gbuf", hf)], writes=[K("ghalo", l, g, hf)])
            for hf, ccs in enumerate(halves):
                for cc in ccs:
                    c = c0 + cc
                    vb, vbk = bank_ffn()
                    proj_fm(wvv, wkv, cc * 128, 128, HK, vb, vbk)
                    gc, gck = scratch()
                    rk = [K("gbuf", hf), K("gbufh", hf), K("pp")]
                    ts(gc[:], gbuf[:, cc, 2:TT + 2], P("fw2", l, c), P("fb", l, c), ALU.mult, ALU.add, reads=rk, writes=[gck])
                    stt(gc[:], gbuf[:, cc, 1:TT + 1], P("fw1", l, c), gc[:], ALU.mult, ALU.add, reads=rk + [gck], writes=[gck])
                    stt(gc[:], gbuf[:, cc, 0:TT], P("fw0", l, c), gc[:], ALU.mult, ALU.add, reads=rk + [gck], writes=[gck])
                    act(gc[:], gc[:], AF.Gelu_apprx_tanh, reads=[gck], writes=[gck])
                    tt(ybuf[:, c * TT:(c + 1) * TT], gc[:], vb[:, :], ALU.mult, reads=[gck, vbk], writes=[YK[c]])
        for d in range(8):
            wv, wk, _ = next_weights(("down", l))
            bank, bkey = bank_ffn()
            mms = [dict(out=bank[:, :], lhsT=wv[:, kc, 0:128], rhs=ybuf[:, kc * TT:(kc + 1) * TT], start=(kc == 0), stop=(kc == NFF - 1)) for kc in range(NFF)]
            mm_group(mms, reads=[wk] + YK, writes=[bkey])
            tt(xT[:, d, :], xT[:, d, :], bank[:, :], ALU.add, reads=[K("xT", d), bkey], writes=[K("xT", d)])

    for t in range(NT):
        load_tile(t)
        for l in range(2):
            dbg_state["on"] = (t == DBG_T and l == 0)
            mixer(l)
            for c in range(8):
                tap(c, oT[:, c, :], [K("oT", c)])
                tap(8 + c, xT[:, c, :], [K("xT", c)])
            ffn(l)
            for c in range(8):
                tap(16 + c, xT[:, c, :], [K("xT", c)])
            dbg_state["on"] = False
        outs = rmsnorm(lambda c: pp[:, G_NF + c:G_NF + c + 1], final=True)
        store_tile(t, outs)
    S.add("sp", None, reads=[K("outd", t * TT + blk * 128) for t in range(NT) for blk in range(4)] + ([K("dbgd", i) for i in range(dbg)] if dbg else []))
    assert wstate["next_use"] == len(stream)
    S.emit(nc, es)
    es.close()
    return nc


_CACHE = {}


def _group_list():
    gs = []
    for g in range(6):
        c0 = g * 512
        gs.append(("w_in", c0, min(512, NCI - c0)))
    for g in range(2):
        gs.append(("w_out", g * 512, 512))
    for g in range(6):
        c0 = g * 512
        n = min(512, DFF - c0)
        gs.append(("ffn_w_gate", c0, n))
        gs.append(("ffn_w_val", c0, n))
    for dt in range(8):
        gs.append(("ffn_w_down", dt * 128, 128))
    return gs


def _host_inputs(inp):
    pp = _pack_params(inp)
    cm = _const_mat()
    rgw = np.ascontiguousarray(np.stack([inp["rg_w_a"], inp["rg_w_x"]], axis=1).astype(np.float32))
    gl = _group_list()
    assert len(gl) == NGRP
    wall = np.zeros((2 * NGRP, 128, SLOT), np.float32)
    for l in range(2):
        for gi, (name, c0, n) in enumerate(gl):
            w = np.asarray(inp[name][l], np.float32)
            kk = w.shape[0] // 128
            blk = w[:, c0:c0 + n].reshape(kk, 128, n).transpose(1, 0, 2).reshape(128, kk * n)
            wall[l * NGRP + gi, :, 0:kk * n] = blk
    com = {
        "wall": wall,
        "pp": pp, "cm": cm, "rgw": rgw,
        "wgup": np.ascontiguousarray(inp["gla_w_gate_up"], dtype=np.float32),
    }
    return com


def run(inp, n_cores=None, dbg=0):
    x = np.asarray(inp["x"], dtype=np.float32)
    B, T, _ = x.shape
    n_cores = B if n_cores is None else n_cores
    nc = build(T, dbg)
    com = _host_inputs(inp)
    in_maps = []
    for b in range(B):
        m = dict(com)
        m["x"] = np.ascontiguousarray(x[b])
        in_maps.append(m)
    res = run_bass_kernel_spmd(nc, in_maps, core_ids=list(range(B)))
    if dbg:
        return np.stack([np.asarray(r["out"], dtype=np.float32) for r in res.results], axis=0), [np.asarray(r["dbg"]) for r in res.results]
    return np.stack([np.asarray(r["out"], dtype=np.float32) for r in res.results], axis=0)


def kernel(**inputs):
    inp = {k: np.asarray(v) for k, v in inputs.items()}
    return run(inp)
```

```python
import numpy as np
from contextlib import ExitStack
import concourse.bass as bass
import concourse.mybir as mybir
from concourse.bass_utils import run_bass_kernel_spmd

F32 = mybir.dt.float32
BF16 = mybir.dt.bfloat16
AF = mybir.ActivationFunctionType
ALU = mybir.AluOpType

D = 1024
TT = 512
NCI = 2832
DFF = 2688
NFF = 21
EPS = 1e-6
NSLOT = 4
DBG_T = 0
SLOT = 4096

_PL = {}
_off = 0
for _n, _c in [("norm_mix", 8), ("hgrn_norm", 2), ("cw0", 4), ("cw1", 4), ("cw2", 4), ("cw3", 4),
               ("conv_b", 4), ("b_a", 4), ("b_x", 4), ("lam", 4), ("gla_b", 1), ("gla_norm", 2),
               ("norm_ffn", 8), ("fw0", NFF), ("fw1", NFF), ("fw2", NFF), ("fb", NFF)]:
    _PL[_n] = (_off, _c)
    _off += _c
LP = _off
G_NF = 2 * LP
G_LB0 = G_NF + 8
G_LB1 = G_LB0 + 2
NP_COLS = G_LB1 + 2
C_ID = 0
C_M64 = 128
C_BDA = 192
C_BDC = 320
C_ONE = 576
NC_COLS = C_ONE + 512


def _fm(v):
    v = np.asarray(v, np.float32)
    return np.ascontiguousarray(v.reshape(-1, 128).T)


def _pack_params(inp):
    pp = np.zeros((128, NP_COLS), np.float32)
    for l in range(2):
        def put(name, v):
            o, c = _PL[name]
            pp[:, l * LP + o: l * LP + o + c] = _fm(v)
        put("norm_mix", inp["norm_mix"][l]); put("hgrn_norm", inp["hgrn_norm"][l])
        for k in range(4):
            put("cw%d" % k, inp["rg_conv_w"][l, k])
        put("conv_b", inp["rg_conv_b"][l]); put("b_a", inp["rg_b_a"][l]); put("b_x", inp["rg_b_x"][l])
        put("lam", inp["rg_lambda"][l]); put("gla_b", inp["gla_b_gate"][l]); put("gla_norm", inp["gla_norm"][l])
        put("norm_ffn", inp["norm_ffn"][l])
        for k in range(3):
            put("fw%d" % k, inp["ffn_conv_w"][l, k])
        put("fb", inp["ffn_conv_b"][l])
    pp[:, G_NF:G_NF + 8] = _fm(inp["norm_final"])
    pp[:, G_LB0:G_LB0 + 2] = _fm(inp["hgrn_lower_bounds"][0])
    pp[:, G_LB1:G_LB1 + 2] = _fm(inp["hgrn_lower_bounds"][1])
    return pp


def _const_mat():
    cm = np.zeros((128, NC_COLS), np.float32)
    p = np.arange(128)[:, None]
    cm[:, C_ID:C_ID + 128] = (p == np.arange(128)[None, :])
    cm[:, C_M64:C_M64 + 64] = ((p % 64) <= np.arange(64)[None, :])
    cm[:, C_BDA:C_BDA + 128] = ((p // 64) == (np.arange(128)[None, :] // 64))
    cm[:, C_BDC:C_BDC + 256] = ((p // 32) == (np.arange(256)[None, :] // 64))
    cm[:, C_ONE:C_ONE + 512] = 1.0
    return cm


class _Op:
    __slots__ = ("eng", "fn", "deps", "dma", "signal", "sem", "val", "n")


class Sched:
    def __init__(self):
        self.ops = []
        self.lastw = {}
        self.readers = {}

    cap = None

    def begin(self):
        self.cap = []

    def end(self):
        c = self.cap
        self.cap = None
        return c

    def merge(self, A, B):
        i = j = 0
        while i < len(A) or j < len(B):
            fa = i / max(len(A), 1)
            fb = j / max(len(B), 1)
            if j >= len(B) or (i < len(A) and fa <= fb):
                self.add(*A[i]); i += 1
            else:
                self.add(*B[j]); j += 1

    def add(self, eng, fn, reads=(), writes=(), dma=None):
        if self.cap is not None:
            self.cap.append((eng, fn, list(reads), list(writes), dma))
            return None
        op = _Op()
        op.eng = eng; op.fn = fn; op.dma = dma; op.signal = False; op.sem = None; op.val = 0
        deps = set()
        for k in reads:
            w = self.lastw.get(k)
            if w is not None:
                deps.add(w)
        for k in writes:
            w = self.lastw.get(k)
            if w is not None:
                deps.add(w)
            for r in self.readers.get(k, ()):
                deps.add(r)
        op.deps = [d for d in deps if not (d.dma is None and op.dma is None and d.eng == "pe" and eng == "pe")]
        for k in reads:
            self.readers.setdefault(k, []).append(op)
        for k in writes:
            self.lastw[k] = op
            self.readers[k] = []
        self.ops.append(op)
        return op

    def emit(self, nc, es):
        for op in self.ops:
            for d in op.deps:
                d.signal = True
        cnt = {}
        for op in self.ops:
            if not op.signal:
                continue
            if op.dma is not None:
                name = "d_" + op.dma
                cnt[name] = cnt.get(name, 0) + 16
            else:
                name = "e_" + op.eng
                cnt[name] = cnt.get(name, 0) + 1
            op.sem = name
            op.val = cnt[name]
        for nm in ("d_const",):
            if nm in cnt:
                for op in self.ops:
                    if op.sem == nm:
                        op.val = cnt[nm]
        sems = {name: es.enter_context(nc.semaphore(name)) for name in cnt}
        block = es.enter_context(nc.Block())
        engs = {"pe": block.tensor, "act": block.scalar, "dve": block.vector, "pool": block.gpsimd, "sp": block.sync}
        for ename, deco in engs.items():
            mine = [op for op in self.ops if op.eng == ename]

            def body(e, mine=mine):
                waited = {}
                for op in mine:
                    need = {}
                    for d in op.deps:
                        if d.val > need.get(d.sem, 0):
                            need[d.sem] = d.val
                    for s, v in need.items():
                        if v > waited.get(s, 0):
                            e.wait_ge(sems[s], v)
                            waited[s] = v
                    if op.fn is None:
                        continue
                    ins = op.fn(e)
                    if op.signal:
                        ins.then_inc(sems[op.sem], 16 if op.dma is not None else 1)
            deco(body)


def build(T, dbg=0):
    NT = T // TT
    nc = bass.Bass("TRN2", target_bir_lowering=False)
    x_d = nc.dram_tensor("x", [T, D], F32, kind="ExternalInput").ap()
    out_d = nc.dram_tensor("out", [T, D], F32, kind="ExternalOutput").ap()
    w_in_d = nc.dram_tensor("w_in", [2, D, NCI], F32, kind="ExternalInput").ap()
    w_out_d = nc.dram_tensor("w_out", [2, D, D], F32, kind="ExternalInput").ap()
    w_gate_d = nc.dram_tensor("w_gate", [2, D, DFF], F32, kind="ExternalInput").ap()
    w_val_d = nc.dram_tensor("w_val", [2, D, DFF], F32, kind="ExternalInput").ap()
    w_down_d = nc.dram_tensor("w_down", [2, DFF, D], F32, kind="ExternalInput").ap()
    pp_d = nc.dram_tensor("pp", [128, NP_COLS], F32, kind="ExternalInput").ap()
    cm_d = nc.dram_tensor("cm", [128, NC_COLS], F32, kind="ExternalInput").ap()
    rgw_d = nc.dram_tensor("rgw", [2, 2, 8, 64, 64], F32, kind="ExternalInput").ap()
    wgup_d = nc.dram_tensor("wgup", [2, 16, 128], F32, kind="ExternalInput").ap()

    S = Sched()
    es = ExitStack()
    dbg_d = nc.dram_tensor("dbg", [max(dbg, 1), 128, TT], F32, kind="ExternalOutput").ap() if dbg else None
    dbg_state = {"on": False}

    def tap(idx, ap, keys):
        if dbg and dbg_state["on"]:
            S.add("pool", lambda e: e.dma_start(out=dbg_d[idx], in_=ap), reads=keys, writes=[K("dbgd", idx)], dma="dbg")

    def sb(name, shape, dt):
        return es.enter_context(nc.sbuf_tensor("s_" + name, shape, dt))

    pp = sb("pp", [128, NP_COLS], F32)
    dv = sb("dv", [128, 64], F32)
    cm = sb("cm", [128, NC_COLS], F32)
    ident_bf = sb("ident_bf", [128, 128], BF16)
    ones_bf = sb("ones_bf", [128, 128], BF16)
    bda_bf = sb("bda_bf", [128, 128], BF16)
    wbd = sb("wbd", [128, 16, 128], BF16)
    wgup = sb("wgup", [16, 2, 128], F32)
    xin = [sb("xin%d" % i, [128, D], F32) for i in range(2)]
    xout = [sb("xout%d" % i, [128, D], F32) for i in range(2)]
    xT = sb("xT", [128, 8, TT], F32)
    hT = sb("hT", [128, 8, TT], BF16)
    ybuf = sb("ybuf", [128, NFF * TT], BF16)
    ybuf_f = ybuf.bitcast(F32)
    NSC = 10
    NSCB = 8
    NSCD = 4
    sc_all = sb("sc_all", [128, (NSC + NSCB + NSCD) * TT], F32)
    sc = [sc_all[:, i * TT:(i + 1) * TT] for i in range(NSC + NSCB + NSCD)]
    SB = sb("SB", [128, 8 * 128], BF16)
    explast0 = sb("explast0", [128, 8], F32)
    ngx = sb("ngx", [128, TT + 4], F32)
    bx = [[sb("bx%d_%d" % (l, j), [128, TT + 4], F32) for j in range(4)] for l in range(2)]
    hcar = sb("hcar", [128, 8], F32)
    qT = sb("qT", [128, TT], BF16)
    kT = sb("kT", [128, TT], BF16)
    khT = sb("khT", [128, TT], BF16)
    xcb = sb("xcb", [128, TT], BF16)
    osq = sb("osq", [128, TT], BF16)
    clr = sb("clr", [16, TT], F32)
    explast = sb("explast", [128, 8], F32)
    v_tm = sb("v_tm", [128, 4 * 256], BF16)
    vc_tm = sb("vc_tm", [128, 4 * 256], BF16)
    khat = sb("khat", [128, 4 * 128], BF16)
    sT = [sb("sT%d" % i, [128, 4 * 128], BF16) for i in range(4)]
    S32 = [[[sb("S32_%d_%d_%d" % (l, u, v), [128, 256 if u == 2 else 128], F32) for v in range(1)] for u in range(3)] for l in range(2)]
    Sbf = [[[sb("Sbf_%d_%d_%d" % (l, u, v), [128, 256 if u == 2 else 128], BF16) for v in range(1)]
            for u in range(3)] for l in range(2)]
    oT = sb("oT", [128, 8, TT], BF16)
    gbuf = sb("gbuf", [128, 4, TT + 2], F32)
    ghalo = sb("ghalo", [128, 2, NFF + 3, 2], F32)
    wbd_f = gbuf[:].rearrange("p a b -> p (a b)")[:, 0:2048].rearrange("p (a b) -> p a b", b=128)
    wslot = [sb("wslot%d" % i, [128, SLOT], BF16) for i in range(NSLOT)]
    banks = [es.enter_context(nc.psum_tensor("bank%d" % i, [128, TT], F32)) for i in range(8)]

    def K(*a):
        return tuple(a)

    YK = [K("y", c) for c in range(NFF)]

    def gate_view(g):
        return ybuf_f[:, g * TT:(g + 1) * TT], [YK[2 * g], YK[2 * g + 1]]

    def sq_view(c):
        return ybuf[:, c * TT:(c + 1) * TT], [YK[c]]

    def P(name, l, j=0):
        o, c = _PL[name]
        return pp[:, l * LP + o + j: l * LP + o + j + 1]

    DV_LB = 0
    DV_L1M = 4
    DV_M8 = 8
    DV_M16 = 16
    DV_NGB = 24
    DV_T = 32

    S.add("sp", lambda e: e.dma_start(out=pp[:], in_=pp_d), writes=[K("pp")], dma="const")
    S.add("sp", lambda e: e.dma_start(out=cm[:], in_=cm_d), writes=[K("cm")], dma="const")
    S.add("sp", lambda e: e.dma_start(out=wgup[:], in_=wgup_d.rearrange("l k n -> k l n")), writes=[K("wgup")], dma="const")
    S.add("dve", lambda e: e.memset(wbd_f, 0.0), writes=[K("wbd_f", i) for i in range(32)] + [K("gbuf", 0), K("gbuf", 1), K("gbufh", 0), K("gbufh", 1)])
    for l in range(2):
        for ax in range(2):
            for blk in range(8):
                j, hh = blk // 2, blk % 2
                idx = (l * 2 + ax) * 4 + j
                S.add("sp", lambda e, l=l, ax=ax, blk=blk, idx=idx, hh=hh: e.dma_start(
                    out=wbd_f[hh * 64:(hh + 1) * 64, idx, hh * 64:(hh + 1) * 64], in_=rgw_d[l, ax, blk]),
                    reads=[], writes=[K("wbd_f", (l * 2 + ax) * 8 + blk)], dma="const")
    S.add("dve", lambda e: e.tensor_copy(out=wbd[:], in_=wbd_f), reads=[K("wbd_f", i) for i in range(32)] + [K("gbuf", 0), K("gbuf", 1), K("gbufh", 0), K("gbufh", 1)], writes=[K("wbd")])
    S.add("dve", lambda e: e.tensor_copy(out=ident_bf[:], in_=cm[:, C_ID:C_ID + 128]), reads=[K("cm")], writes=[K("ident_bf")])
    S.add("dve", lambda e: e.tensor_copy(out=ones_bf[:], in_=cm[:, C_ONE:C_ONE + 128]), reads=[K("cm")], writes=[K("ones_bf")])
    S.add("dve", lambda e: e.tensor_copy(out=bda_bf[:], in_=cm[:, C_BDA:C_BDA + 128]), reads=[K("cm")], writes=[K("bda_bf")])
    ident = cm[:, C_ID:C_ID + 128]
    mask64 = cm[:, C_M64:C_M64 + 64]
    bdA = cm[:, C_BDA:C_BDA + 128]
    bdC = cm[:, C_BDC:C_BDC + 256]
    ones_f = cm[:, C_ONE:C_ONE + TT]
    S.add("dve", lambda e: e.memset(dv[:], 0.0), writes=[K("dv")])
    S.add("dve", lambda e: e.tensor_tensor(out=dv[:, DV_T:DV_T + 2], in0=pp[:, G_LB1:G_LB1 + 2], in1=pp[:, G_LB0:G_LB0 + 2], op=ALU.subtract),
          reads=[K("pp"), K("dv")], writes=[K("dvt")])
    S.add("act", lambda e: e.activation(out=dv[:, DV_LB + 2:DV_LB + 4], in_=dv[:, DV_T:DV_T + 2], func=AF.Sigmoid),
          reads=[K("dvt"), K("dv")], writes=[K("dvlb")])
    S.add("act", lambda e: e.activation(out=dv[:, DV_L1M:DV_L1M + 4], in_=dv[:, DV_LB:DV_LB + 4], func=AF.Ln, scale=-1.0, bias=1.0),
          reads=[K("dvlb")], writes=[K("dvl1m")])
    for l in range(2):
        o = _PL["lam"][0] + l * LP
        S.add("act", lambda e, o=o, l=l: e.activation(out=dv[:, DV_T + 4 + 4 * l:DV_T + 8 + 4 * l], in_=pp[:, o:o + 4], func=AF.Exp, scale=-1.0),
              reads=[K("pp"), K("dv")], writes=[K("dvt2", l)])
        S.add("act", lambda e, l=l: e.activation(out=dv[:, DV_T + 12 + 4 * l:DV_T + 16 + 4 * l], in_=dv[:, DV_T + 4 + 4 * l:DV_T + 8 + 4 * l], func=AF.Ln, bias=1.0),
              reads=[K("dvt2", l)], writes=[K("dvt3", l)])
        S.add("dve", lambda e, l=l: e.tensor_scalar(out=dv[:, DV_M8 + 4 * l:DV_M8 + 4 * l + 4], in0=dv[:, DV_T + 12 + 4 * l:DV_T + 16 + 4 * l], scalar1=-8.0, scalar2=None, op0=ALU.mult),
              reads=[K("dvt3", l)], writes=[K("dvm8", l)])
        S.add("dve", lambda e, l=l: e.tensor_scalar(out=dv[:, DV_M16 + 4 * l:DV_M16 + 4 * l + 4], in0=dv[:, DV_T + 12 + 4 * l:DV_T + 16 + 4 * l], scalar1=-16.0, scalar2=None, op0=ALU.mult),
              reads=[K("dvt3", l)], writes=[K("dvm16", l)])
        og = _PL["gla_b"][0] + l * LP
        S.add("dve", lambda e, l=l, og=og: e.tensor_scalar(out=dv[:, DV_NGB + l:DV_NGB + l + 1], in0=pp[:, og:og + 1], scalar1=-1.0, scalar2=None, op0=ALU.mult),
              reads=[K("pp"), K("dv")], writes=[K("dvngb", l)])
    DVK = [K("dvlb"), K("dvl1m")] + [K(n, l) for n in ("dvm8", "dvm16", "dvngb") for l in range(2)]
    for l in range(2):
        for u in range(3):
            S.add("dve", lambda e, l=l, u=u: e.memset(S32[l][u][0][:], 0.0), writes=[K("S32", l, u, 0), K("S32", l, u, 1)])
            S.add("dve", lambda e, l=l, u=u: e.memset(Sbf[l][u][0][:], 0.0), writes=[K("Sbf", l, u, 0), K("Sbf", l, u, 1)])
        for j in range(4):
            S.add("dve", lambda e, l=l, j=j: e.memset(bx[l][j][:], 0.0), writes=[K("bx", l, j)])
    S.add("dve", lambda e: e.memset(hcar[:], 0.0), writes=[K("hcar", 0), K("hcar", 1)])
    S.add("dve", lambda e: e.memset(ghalo[:], 0.0), writes=[K("ghalo", l_, g_, h_) for l_ in range(2) for g_ in range(6) for h_ in range(2)])
    S.add("dve", lambda e: e.memset(ngx[:], 0.0), writes=[K("ngx")])
    for i in range(4):
        S.add("dve", lambda e, i=i: e.memset(sT[i][:], 0.0), writes=[K("sT", i)])

    def layer_groups(l):
        gs = []
        for g in range(6):
            c0 = g * 512
            n = min(512, NCI - c0)
            gs.append(("in", l, c0, n))
        for g in range(2):
            gs.append(("out", l, g * 512, 512))
        for g in range(6):
            c0 = g * 512
            n = min(512, DFF - c0)
            gs.append(("gate", l, c0, n))
            gs.append(("val", l, c0, n))
        for dt in range(8):
            gs.append(("down", l, dt * 128, 128))
        return gs

    stream = []
    for t in range(NT):
        for l in range(2):
            stream += layer_groups(l)
    wstate = {"next_load": 0, "next_use": 0}

    def issue_load(i):
        kind, l, c0, n = stream[i]
        s = i % NSLOT
        if kind == "down":
            src = w_down_d[l].rearrange("(k p) n -> p k n", p=128)[:, :, c0:c0 + n]
            dst = wslot[s][:, 0:NFF * n].rearrange("p (k n) -> p k n", k=NFF)
        else:
            wd = {"in": w_in_d, "out": w_out_d, "gate": w_gate_d, "val": w_val_d}[kind]
            src = wd[l].rearrange("(k p) n -> p k n", p=128)[:, :, c0:c0 + n]
            dst = wslot[s][:, 0:8 * n].rearrange("p (k n) -> p k n", k=8)
        S.add("pool", lambda e, src=src, dst=dst: e.dma_start(out=dst, in_=src), writes=[K("wslot", s)], dma="w%d" % s)

    def next_weights(expect):
        i = wstate["next_use"]
        while wstate["next_load"] <= min(len(stream) - 1, i + NSLOT - 2):
            issue_load(wstate["next_load"])
            wstate["next_load"] += 1
        kind, l, c0, n = stream[i]
        assert kind == expect[0] and l == expect[1], (stream[i], expect)
        s = i % NSLOT
        kk = NFF if kind == "down" else 8
        view = wslot[s][:, 0:kk * n].rearrange("p (k n) -> p k n", k=kk)
        wstate["next_use"] += 1
        return view, K("wslot", s), n

    rot = {"proj": 0, "psu": 0, "ffn": 0, "mod": 3}

    def bank_proj():
        b = rot["proj"] % rot["mod"]
        rot["proj"] += 1
        return banks[b], K("bank", b)

    def bank_psu():
        b = 6 + rot["psu"] % 2
        rot["psu"] += 1
        return banks[b], K("bank", b)

    def bank_ffn():
        b = rot["ffn"] % 8
        rot["ffn"] += 1
        return banks[b], K("bank", b)

    sci = {"i": 0}

    def scratch():
        i = sci["i"] % NSC
        sci["i"] += 1
        return sc[i], K("sc", i)

    scbi = {"i": 0}

    def scratchB():
        i = NSC + scbi["i"] % NSCB
        scbi["i"] += 1
        return sc[i], K("sc", i)

    def scratch2():
        if sci["i"] % 2:
            sci["i"] += 1
        i = sci["i"] % NSC
        sci["i"] += 2
        return sc_all[:, i * TT:(i + 2) * TT], [K("sc", i), K("sc", i + 1)]

    def act(out, in_, func, reads, writes, bias=None, scale=None):
        kw = {}
        if bias is not None:
            kw["bias"] = bias
        if scale is not None:
            kw["scale"] = scale
        S.add("act", lambda e: e.activation(out=out, in_=in_, func=func, **kw), reads=reads, writes=writes)

    def tt(out, in0, in1, op, reads, writes, eng="dve"):
        S.add(eng, lambda e: e.tensor_tensor(out=out, in0=in0, in1=in1, op=op), reads=reads, writes=writes)

    def ts(out, in0, s1, s2, op0, op1, reads, writes, eng="dve"):
        if s2 is None:
            S.add(eng, lambda e: e.tensor_scalar(out=out, in0=in0, scalar1=s1, scalar2=None, op0=op0), reads=reads, writes=writes)
        else:
            S.add(eng, lambda e: e.tensor_scalar(out=out, in0=in0, scalar1=s1, scalar2=s2, op0=op0, op1=op1), reads=reads, writes=writes)

    def stt(out, in0, scalar, in1, op0, op1, reads, writes, eng="dve"):
        S.add(eng, lambda e: e.scalar_tensor_tensor(out=out, in0=in0, scalar=scalar, in1=in1, op0=op0, op1=op1), reads=reads, writes=writes)

    def mm_group(mms, reads, writes):
        def fn(e):
            ins = None
            for m in mms:
                kw = dict(start=m["start"], stop=m["stop"])
                if m.get("tp") is not None:
                    kw["tile_position"] = m["tp"]
                if m.get("skip"):
                    kw["skip_group_check"] = True
                ins = e.matmul(m["out"], m["lhsT"], m["rhs"], **kw)
            return ins
        S.add("pe", fn, reads=reads, writes=writes)

    def proj_fm(wv, wk, col0, m, reads_h, bank, bkey):
        mms = [dict(out=bank[0:m, :], lhsT=wv[:, kc, col0:col0 + m], rhs=hT[:, kc, :], start=(kc == 0), stop=(kc == 7)) for kc in range(8)]
        mm_group(mms, reads=[wk] + reads_h, writes=[bkey])

    HK = [K("hT")]

    def rmsnorm(gcol, out_bf16=True, final=False):
        bank, bkey = bank_proj()
        for c in range(8):
            sqv, sqk = sq_view(c)
            act(sqv, xT[:, c, :], AF.Square, reads=[K("xT", c)], writes=sqk)
        mms = []
        rk = []
        for c in range(8):
            sqv, sqk = sq_view(c)
            mms.append(dict(out=bank[:, :], lhsT=ones_bf[:], rhs=sqv, start=(c == 0), stop=(c == 7)))
            rk += sqk
        mm_group(mms, reads=rk + [K("ones_bf")], writes=[bkey])
        lnv, lk = scratch()
        act(lnv[:], bank[:, :], AF.Ln, reads=[bkey], writes=[lk], scale=1.0 / D, bias=EPS)
        rs, rsk = scratch()
        act(rs[:], lnv[:], AF.Exp, reads=[lk], writes=[rsk], scale=-0.5)
        outs = []
        for c in range(8):
            if final:
                o_, ok = scratch()
                stt(o_[:], xT[:, c, :], gcol(c), rs[:], ALU.mult, ALU.mult, reads=[K("xT", c), rsk, K("pp")], writes=[ok])
                outs.append((o_, ok))
            else:
                stt(hT[:, c, :], xT[:, c, :], gcol(c), rs[:], ALU.mult, ALU.mult, reads=[K("xT", c), rsk, K("pp")], writes=HK)
        return outs

    def cumdecay(nlf_ap, nlfk):
        S.add("dve", lambda e: e.tensor_tensor_scan(out=ngx[:, 1:TT + 1], data0=ones_f, data1=nlf_ap, initial=0.0, op0=ALU.mult, op1=ALU.add),
              reads=[nlfk, K("cm")], writes=[K("ngx")])
        cl, clk = scratch()
        cl3 = cl[:].rearrange("p (c i) -> p c i", i=64)
        n3 = ngx[:, 1:TT + 1].rearrange("p (c i) -> p c i", i=64)
        b3 = ngx[:, 0:TT].rearrange("p (c i) -> p c i", i=64)[:, :, 0:1].to_broadcast([128, 8, 64])
        tt(cl3, n3, b3, ALU.subtract, reads=[K("ngx")], writes=[clk])
        return cl, clk

    def khat_transposes(dstkey):
        bank, bkey = bank_proj()
        bb = bank.bitcast(BF16)
        def fn(e):
            ins = None
            for blk in range(4):
                ins = e.transpose(out=bb[:, blk * 128:(blk + 1) * 128], in_=khT[:, blk * 128:(blk + 1) * 128], identity=ident_bf[:])
            return ins
        S.add("pe", fn, reads=[K("khT"), K("ident_bf")], writes=[bkey])
        act(khat[:], bb[:, 0:512], AF.Copy, reads=[bkey], writes=[dstkey])

    def gla_unit(l, u, vbuf, vkey, vcol0, nv, heads, pairs, bdm):
        for hi, (rb, dk, vc, pi, hh) in enumerate(heads):
            sb_i = 3
            bank, bkey = banks[sb_i], K("bank", sb_i)
            mms = []
            for blk in range(4):
                mms.append(dict(out=bank[:, blk * 128:(blk + 1) * 128], lhsT=kT[rb:rb + dk, blk * 128:(blk + 1) * 128],
                                rhs=qT[rb:rb + dk, blk * 128:(blk + 1) * 128], start=True, stop=True,
                                tp=(rb, 0) if rb == 96 else None))
            mm_group(mms, reads=[K("kT"), K("qT")], writes=[bkey])
            b3 = bank[:, :].rearrange("p (b i) -> p b i", i=128)
            s3 = sT[hi][:].rearrange("p (b i) -> p b i", i=128)
            for q in range(2):
                tt(s3[q * 64:(q + 1) * 64, :, q * 64:(q + 1) * 64], b3[q * 64:(q + 1) * 64, :, q * 64:(q + 1) * 64],
                   mask64[q * 64:(q + 1) * 64, :].unsqueeze(1).to_broadcast([64, 4, 64]), ALU.mult,
                   reads=[bkey, K("cm")], writes=[K("sT", hi)])
        if u == 0:
            tap(29, sT[0][:], [K("sT", 0)]); tap(30, sT[1][:], [K("sT", 1)])
        pob = [(banks[4 + pi], K("bank", 4 + pi)) for pi in range(len(pairs))]
        for pi in range(len(pairs)):
            po, pok = pob[pi]
            mms = []
            rk = [vkey]
            for hi, (rb, dk, vc, hpi, hh) in enumerate(heads):
                if hpi != pi:
                    continue
                rk.append(K("sT", hi))
                for blk in range(4):
                    mms.append(dict(out=po[hh * 64:(hh + 1) * 64, blk * 128:(blk + 1) * 128],
                                    lhsT=vbuf[:, blk * 256 + vc: blk * 256 + vc + 64],
                                    rhs=sT[hi][:, blk * 128:(blk + 1) * 128], start=(blk == 0), stop=False, skip=True, tp=(0, 64) if hh == 1 else None))
            mm_group(mms, reads=rk, writes=[pok])
        ex0 = explast0
        S.add("dve", lambda e: e.tensor_copy(out=ex0[:], in_=explast[:]), reads=[K("explast")], writes=[K("ex0")])
        S.add("dve", lambda e: e.memset(ex0[:, 0:1], 0.0), reads=[K("ex0")], writes=[K("ex0")])
        E8, E8k = scratch2()
        E83 = E8.rearrange("p (v c) -> p v c", c=8)
        S.add("dve", lambda e: e.tensor_copy(out=E83, in_=ex0[:].unsqueeze(1).to_broadcast([128, 128, 8])), reads=[K("ex0")], writes=E8k)
        for g in range(len(pairs)):
            po, pok = pob[g]
            vc0 = vcol0 + g * 128
            s32 = S32[l][u][0][:, g * 128:(g + 1) * 128]
            sbf0 = Sbf[l][u][0][:, g * 128:(g + 1) * 128]
            s32k = K("S32", l, u, g)
            sbfk = K("Sbf", l, u, g)
            for b in range(2):
                mms = []
                for cc in range(4):
                    c = 2 * cc + b
                    r0 = (c % 2) * 64
                    blk = c // 2
                    mms.append(dict(out=banks[6 + b][:, cc * 128:(cc + 1) * 128], lhsT=khat[r0:r0 + 64, blk * 128:(blk + 1) * 128],
                                    rhs=vbuf[r0:r0 + 64, blk * 256 + vc0: blk * 256 + vc0 + 128], start=True, stop=True))
                mm_group(mms, reads=[K("khat"), vkey], writes=[K("bank", 6 + b)])
            PUM, PUMk = scratch2()
            PUM3 = PUM.rearrange("p (v c) -> p v c", c=8)
            PUM4 = PUM.rearrange("p (v cc two) -> p v cc two", cc=4, two=2)
            mk = bdm[:, g * 128:(g + 1) * 128]
            for b in range(2):
                src = banks[6 + b][:, :].rearrange("p (c v) -> p v c", v=128)
                tt(PUM4[:, :, :, b], src, mk.unsqueeze(2).to_broadcast([128, 128, 4]), ALU.mult,
                   reads=[K("bank", 6 + b), K("cm")], writes=PUMk)
            stt(PUM3[:, :, 0], s32, explast[:, 0:1], PUM3[:, :, 0], ALU.mult, ALU.add, reads=[s32k, K("explast")] + PUMk, writes=PUMk)
            ST, STk = scratch2()
            S.add("dve", lambda e, ST=ST, E8=E8, PUM=PUM: e.tensor_tensor_scan(out=ST, data0=E8, data1=PUM, initial=0.0, op0=ALU.mult, op1=ALU.add),
                  reads=E8k + PUMk, writes=STk)
            ST3 = ST.rearrange("p (v c) -> p v c", c=8)
            mm_group([dict(out=po[:, 0:64], lhsT=sbf0, rhs=qT[:, 0:64], start=False, stop=True, skip=True)],
                     reads=[sbfk, K("qT"), pok], writes=[pok])
            S.add("dve", lambda e, ST3=ST3: e.tensor_copy(out=SB[:].rearrange("p (c v) -> p v c", v=128), in_=ST3), reads=STk, writes=[K("SB")])
            S.add("dve", lambda e, s32=s32, ST3=ST3: e.tensor_copy(out=s32, in_=ST3[:, :, 7]), reads=STk, writes=[s32k])
            mms = []
            for c in range(1, 8):
                mms.append(dict(out=po[:, c * 64:(c + 1) * 64], lhsT=SB[:, (c - 1) * 128:c * 128], rhs=qT[:, c * 64:(c + 1) * 64],
                                start=False, stop=True, skip=True))
            mm_group(mms, reads=[K("SB"), K("qT"), pok], writes=[pok])
            S.add("dve", lambda e, sbf0=sbf0: e.tensor_copy(out=sbf0, in_=SB[:, 7 * 128:8 * 128]), reads=[K("SB")], writes=[sbfk])
        for pi, (ochunk, gate_ap, gate_keys, normcol) in enumerate(pairs):
            po, pok = pob[pi]
            act(osq[:], po[:, :], AF.Square, reads=[pok], writes=[K("osq")])
            pn, pnk = bank_proj()
            mm_group([dict(out=pn[:, :], lhsT=bda_bf[:], rhs=osq[:], start=True, stop=True)], reads=[K("osq"), K("bda_bf")], writes=[pnk])
            lnv, lk = scratch()
            act(lnv[:], pn[:, :], AF.Ln, reads=[pnk], writes=[lk], scale=1.0 / 64, bias=EPS)
            rs, rsk = scratch()
            act(rs[:], lnv[:], AF.Exp, reads=[lk], writes=[rsk], scale=-0.5)
            on, onk = scratch()
            tt(on[:], po[:, :], rs[:], ALU.mult, reads=[pok, rsk], writes=[onk])
            if u == 0:
                tap(31, on[:], [onk]); tap(32, rs[:], [rsk])
            stt(oT[:, ochunk, :], on[:], normcol, gate_ap, ALU.mult, ALU.mult, reads=[onk, K("pp")] + gate_keys, writes=[K("oT", ochunk)])

    xblk = {"i": 0}
    oblk = {"i": 0}

    def load_tile(t):
        staged = []
        for blk in range(4):
            i = xblk["i"] % 2
            xblk["i"] += 1
            r0 = t * TT + blk * 128
            S.add("sp", lambda e, i=i, r0=r0: e.dma_start(out=xin[i][:], in_=x_d[r0:r0 + 128, :]), writes=[K("xin", i)], dma="xin%d" % i)
            staged.append(i)
            for half in range(2):
                bank, bkey = bank_ffn()
                def fn(e, i=i, half=half, bank=bank):
                    ins = None
                    for cc in range(4):
                        c = half * 4 + cc
                        ins = e.transpose(out=bank[:, cc * 128:(cc + 1) * 128], in_=xin[i][:, c * 128:(c + 1) * 128], identity=ident)
                    return ins
                S.add("pe", fn, reads=[K("xin", i), K("cm")], writes=[bkey])
                dst = xT[:, half * 4:(half + 1) * 4, blk * 128:(blk + 1) * 128]
                src = bank[:, :].rearrange("p (c i) -> p c i", i=128)
                S.add("act" if half == 0 else "dve",
                      (lambda e, dst=dst, src=src: e.activation(out=dst, in_=src, func=AF.Copy)) if half == 0 else
                      (lambda e, dst=dst, src=src: e.tensor_copy(out=dst, in_=src)),
                      reads=[bkey], writes=[K("xT", c) for c in range(half * 4, half * 4 + 4)])

    def store_tile(t, outs):
        for blk in range(4):
            i = oblk["i"] % 2
            oblk["i"] += 1
            for half in range(2):
                bank, bkey = bank_ffn()
                def fn(e, half=half, bank=bank, blk=blk):
                    ins = None
                    for cc in range(4):
                        c = half * 4 + cc
                        ins = e.transpose(out=bank[:, cc * 128:(cc + 1) * 128], in_=outs[c][0][:, blk * 128:(blk + 1) * 128], identity=ident)
                    return ins
                S.add("pe", fn, reads=[outs[c][1] for c in range(half * 4, half * 4 + 4)] + [K("cm")], writes=[bkey])
                dst = xout[i][:, half * 512:(half + 1) * 512]
                if half == 0:
                    S.add("act", lambda e, dst=dst, bank=bank: e.activation(out=dst, in_=bank[:, :], func=AF.Copy), reads=[bkey], writes=[K("xout", i)])
                else:
                    S.add("dve", lambda e, dst=dst, bank=bank: e.tensor_copy(out=dst, in_=bank[:, :]), reads=[bkey], writes=[K("xout", i)])
            r0 = t * TT + blk * 128
            S.add("sp", lambda e, i=i, r0=r0: e.dma_start(out=out_d[r0:r0 + 128, :], in_=xout[i][:]), reads=[K("xout", i)], writes=[K("outd", r0)], dma="xout%d" % i)

    def mixer(l):
        rmsnorm(lambda c: P("norm_mix", l, c))
        wv0, wk0, _ = next_weights(("in", l))
        wv1, wk1, _ = next_weights(("in", l))
        for half in range(2):
            bank, bkey = bank_proj()
            mms = []
            for bb_ in range(2):
                blk = half * 2 + bb_
                for kc in range(8):
                    mms.append(dict(out=bank[:, bb_ * 256:(bb_ + 1) * 256], lhsT=hT[:, kc, blk * 128:(blk + 1) * 128],
                                    rhs=wv1[:, kc, 0:256], start=(kc == 0), stop=(kc == 7)))
            mm_group(mms, reads=[wk1] + HK, writes=[bkey])
            act(v_tm[:, half * 512:(half + 1) * 512], bank[:, :], AF.Copy, reads=[bkey], writes=[K("v_tm")])
        gateA = []
        for p in range(2):
            bank, bkey = bank_proj()
            proj_fm(wv1, wk1, 256 + p * 128, 128, HK, bank, bkey)
            gv, gk = gate_view(p)
            act(gv, bank[:, :], AF.Silu, reads=[bkey], writes=gk)
            gateA.append((gv, gk))
        aqz = []
        for p in range(2):
            bank, bkey = bank_proj()
            proj_fm(wv0, wk0, p * 128, 128, HK, bank, bkey)
            aq, aqk = sc[NSC + NSCB + 2 * p], K("sc", NSC + NSCB + 2 * p)
            act(aq[:], bank[:, :], AF.Copy, reads=[bkey], writes=[aqk])
            zb, zk_ = bank_proj()
            proj_fm(wv0, wk0, 256 + p * 128, 128, HK, zb, zk_)
            zs, zsk = sc[NSC + NSCB + 2 * p + 1], K("sc", NSC + NSCB + 2 * p + 1)
            act(zs[:], zb[:, :], AF.Copy, reads=[zk_], writes=[zsk])
            aqz.append((aq, aqk, zs, zsk))
        wv2, wk2, _ = next_weights(("in", l))
        for j in range(4):
            bank, bkey = bank_proj()
            proj_fm(wv2, wk2, j * 128, 128, HK, bank, bkey)
            ts(bx[l][j][:, 0:3], bx[l][j][:, TT:TT + 3], 1.0, None, ALU.mult, None, reads=[K("bx", l, j)], writes=[K("bxh", l, j)])
            act(bx[l][j][:, 3:TT + 3], bank[:, :], AF.Copy, reads=[bkey, K("bxh", l, j)], writes=[K("bx", l, j)])
        wv3, wk3, _ = next_weights(("in", l))
        gateB = []
        for j in range(4):
            bank, bkey = bank_proj()
            proj_fm(wv3, wk3, j * 128, 128, HK, bank, bkey)
            gv, gk = gate_view(2 + j)
            act(gv, bank[:, :], AF.Gelu_apprx_tanh, reads=[bkey], writes=gk)
            gateB.append((gv, gk))
        S.begin()
        for p in range(2):
            aq, aqk, zb, zk = aqz[p]
            E, Ek = scratch()
            act(E[:], zb[:], AF.Exp, reads=[zk], writes=[Ek], scale=-1.0)
            L1, L1k = scratch()
            act(L1[:], E[:], AF.Ln, reads=[Ek], writes=[L1k], bias=1.0)
            L2, L2k = scratch()
            act(L2[:], E[:], AF.Ln, reads=[Ek] + DVK, writes=[L2k], bias=1.0, scale=dv[:, DV_LB + 2 * l + p:DV_LB + 2 * l + p + 1])
            W, Wk = scratch()
            tt(W[:], zb[:], L1[:], ALU.add, reads=[zk, L1k], writes=[Wk], eng="pool")
            nlf, nlfk = scratch()
            tt(nlf[:], L1[:], L2[:], ALU.subtract, reads=[L1k, L2k], writes=[nlfk], eng="pool")
            cl, clk = cumdecay(nlf[:], nlfk)
            qe, qek = scratch()
            act(qe[:], cl[:], AF.Exp, reads=[clk], writes=[qek], scale=-1.0)
            tt(qT[:], aq[:], qe[:], ALU.mult, reads=[aqk, qek], writes=[K("qT")])
            ak, akk = scratch()
            tt(ak[:], cl[:], W[:], ALU.subtract, reads=[clk, Wk], writes=[akk], eng="pool")
            act(kT[:], ak[:], AF.Exp, reads=[akk] + DVK, writes=[K("kT")], bias=dv[:, DV_L1M + 2 * l + p:DV_L1M + 2 * l + p + 1])
            cl3 = cl[:].rearrange("p (c i) -> p c i", i=64)
            act(explast[:], cl3[:, :, 63], AF.Exp, reads=[clk], writes=[K("explast")], scale=-1.0)
            tt(khT[:].rearrange("p (c i) -> p c i", i=64), kT[:].rearrange("p (c i) -> p c i", i=64),
               explast[:].unsqueeze(2).to_broadcast([128, 8, 64]), ALU.mult, reads=[K("kT"), K("explast")], writes=[K("khT")])
            khat_transposes(K("khat"))
            if p == 0:
                tap(24, qT[:], [K("qT")]); tap(25, kT[:], [K("kT")]); tap(26, khat[:], [K("khat")]); tap(27, cl[:], [clk])
                tap(28, v_tm[:, 0:512], [K("v_tm")])
            heads = [(0, 64, p * 128, 0, 0), (64, 64, p * 128 + 64, 0, 1)]
            pairs = [(p, gateA[p][0], gateA[p][1], P("hgrn_norm", l, p))]
            gla_unit(l, p, v_tm, K("v_tm"), p * 128, 128, heads, pairs, bdA)
        capA = S.end()
        def rg_chunk(j, rgb):
            b_ = bx[l][j]
            bk = [K("bx", l, j), K("bxh", l, j), K("pp")]
            xc, xck = scratchB()
            ts(xc[:], b_[:, 3:TT + 3], P("cw3", l, j), P("conv_b", l, j), ALU.mult, ALU.add, reads=bk, writes=[xck])
            for k in range(3):
                stt(xc[:], b_[:, k:k + TT], P("cw%d" % k, l, j), xc[:], ALU.mult, ALU.add, reads=bk + [xck], writes=[xck])
            act(xcb[:], xc[:], AF.Copy, reads=[xck], writes=[K("xcb")])
            pr, prk = banks[rgb], K("bank", rgb)
            mm_group([dict(out=pr[:, :], lhsT=wbd[:, (l * 2 + 0) * 4 + j, :], rhs=xcb[:], start=True, stop=True)], reads=[K("xcb"), K("wbd")], writes=[prk])
            r, rk = scratchB()
            act(r[:], pr[:, :], AF.Sigmoid, reads=[prk, K("pp")], writes=[rk], bias=P("b_a", l, j))
            pg, pgk = banks[rgb], K("bank", rgb)
            mm_group([dict(out=pg[:, :], lhsT=wbd[:, (l * 2 + 1) * 4 + j, :], rhs=xcb[:], start=True, stop=True)], reads=[K("xcb"), K("wbd")], writes=[pgk])
            gi, gik = scratchB()
            act(gi[:], pg[:, :], AF.Sigmoid, reads=[pgk, K("pp")], writes=[gik], bias=P("b_x", l, j))
            a, ak_ = scratchB()
            act(a[:], r[:], AF.Exp, reads=[rk] + DVK, writes=[ak_], scale=dv[:, DV_M8 + 4 * l + j:DV_M8 + 4 * l + j + 1])
            a2, a2k = scratchB()
            act(a2[:], r[:], AF.Exp, reads=[rk] + DVK, writes=[a2k], scale=dv[:, DV_M16 + 4 * l + j:DV_M16 + 4 * l + j + 1])
            ts(a2[:], a2[:], 1.0, -1e-12, ALU.subtract, ALU.min, reads=[a2k], writes=[a2k])
            act(a2[:], a2[:], AF.Ln, reads=[a2k], writes=[a2k], scale=-1.0)
            act(a2[:], a2[:], AF.Exp, reads=[a2k], writes=[a2k], scale=0.5)
            tt(gi[:], gi[:], xc[:], ALU.mult, reads=[gik, xck], writes=[gik], eng="pool")
            tt(gi[:], gi[:], a2[:], ALU.mult, reads=[gik, a2k], writes=[gik], eng="pool")
            hh_, hk = scratchB()
            S.add("dve", lambda e, hh_=hh_, a=a, gi=gi, l=l, j=j: e.tensor_tensor_scan(
                out=hh_[:], data0=a[:], data1=gi[:], initial=hcar[:, l * 4 + j:l * 4 + j + 1], op0=ALU.mult, op1=ALU.add),
                reads=[ak_, gik, K("hcar", l)], writes=[hk])
            ts(hcar[:, l * 4 + j:l * 4 + j + 1], hh_[:, TT - 1:TT], 1.0, None, ALU.mult, None, reads=[hk], writes=[K("hcar", l)])
            tt(oT[:, 2 + j, :], hh_[:], gateB[j][0], ALU.mult, reads=[hk] + gateB[j][1], writes=[K("oT", 2 + j)])
        S.begin()
        rg_chunk(0, 5); rg_chunk(1, 5)
        capB1 = S.end()
        S.begin()
        rg_chunk(2, 2); rg_chunk(3, 2)
        capB2 = S.end()
        S.merge(capA, capB1)
        wv4, wk4, _ = next_weights(("in", l))
        for half in range(2):
            bank, bkey = bank_proj()
            mms = []
            for bb_ in range(2):
                blk = half * 2 + bb_
                for kc in range(8):
                    mms.append(dict(out=bank[:, bb_ * 256:(bb_ + 1) * 256], lhsT=hT[:, kc, blk * 128:(blk + 1) * 128],
                                    rhs=wv4[:, kc, 256:512], start=(kc == 0), stop=(kc == 7)))
            mm_group(mms, reads=[wk4] + HK, writes=[bkey])
            act(vc_tm[:, half * 512:(half + 1) * 512], bank[:, :], AF.Copy, reads=[bkey], writes=[K("vc_tm")])
        bank, bkey = bank_proj()
        proj_fm(wv4, wk4, 0, 128, HK, bank, bkey)
        cq, cqk = scratch()
        act(cq[:], bank[:, :], AF.Copy, reads=[bkey], writes=[cqk], scale=32.0 ** -0.5)
        bank, bkey = bank_proj()
        proj_fm(wv4, wk4, 128, 128, HK, bank, bkey)
        ck, ckk = scratch()
        act(ck[:], bank[:, :], AF.Copy, reads=[bkey], writes=[ckk])
        wv5, wk5, _ = next_weights(("in", l))
        gateC = []
        for p in range(2):
            bank, bkey = bank_proj()
            proj_fm(wv5, wk5, p * 128, 128, HK, bank, bkey)
            gv, gk = gate_view(6 + p)
            act(gv, bank[:, :], AF.Silu, reads=[bkey], writes=gk)
            gateC.append((gv, gk))
        bank, bkey = bank_proj()
        proj_fm(wv5, wk5, 256, 16, HK, bank, bkey)
        act(clr[:], bank[0:16, :], AF.Copy, reads=[bkey], writes=[K("clr")])
        S.begin()
        rot["mod"] = 2
        pgb, pgbk = bank_proj()
        mm_group([dict(out=pgb[:, :], lhsT=wgup[:, l, :], rhs=clr[:], start=True, stop=True)], reads=[K("clr"), K("wgup")], writes=[pgbk])
        E, Ek = scratch()
        act(E[:], pgb[:, :], AF.Exp, reads=[pgbk] + DVK, writes=[Ek], scale=-1.0, bias=dv[:, DV_NGB + l:DV_NGB + l + 1])
        L, Lk = scratch()
        act(L[:], E[:], AF.Ln, reads=[Ek], writes=[Lk], bias=1.0)
        cl, clk = cumdecay(L[:], Lk)
        qe, qek = scratch()
        act(qe[:], cl[:], AF.Exp, reads=[clk], writes=[qek], scale=-1.0 / 16)
        tt(qT[:], cq[:], qe[:], ALU.mult, reads=[cqk, qek], writes=[K("qT")])
        ke, kek = scratch()
        act(ke[:], cl[:], AF.Exp, reads=[clk], writes=[kek], scale=1.0 / 16)
        tt(kT[:], ck[:], ke[:], ALU.mult, reads=[ckk, kek], writes=[K("kT")])
        cl3 = cl[:].rearrange("p (c i) -> p c i", i=64)
        act(explast[:], cl3[:, :, 63], AF.Exp, reads=[clk], writes=[K("explast")], scale=-1.0 / 16)
        tt(khT[:].rearrange("p (c i) -> p c i", i=64), kT[:].rearrange("p (c i) -> p c i", i=64),
           explast[:].unsqueeze(2).to_broadcast([128, 8, 64]), ALU.mult, reads=[K("kT"), K("explast")], writes=[K("khT")])
        khat_transposes(K("khat"))
        heads = [(32 * h, 32, 64 * h, h // 2, h % 2) for h in range(4)]
        pairs = [(6 + p, gateC[p][0], gateC[p][1], P("gla_norm", l, p)) for p in range(2)]
        gla_unit(l, 2, vc_tm, K("vc_tm"), 0, 256, heads, pairs, bdC)
        rot["mod"] = 3
        capC = S.end()
        S.merge(capC, capB2)
        for g in range(2):
            wv, wk, _ = next_weights(("out", l))
            for dt in range(4):
                d = g * 4 + dt
                bank, bkey = bank_proj()
                mms = [dict(out=bank[:, :], lhsT=wv[:, kc, dt * 128:(dt + 1) * 128], rhs=oT[:, kc, :], start=(kc == 0), stop=(kc == 7)) for kc in range(8)]
                mm_group(mms, reads=[wk] + [K("oT", kc) for kc in range(8)], writes=[bkey])
                tt(xT[:, d, :], xT[:, d, :], bank[:, :], ALU.add, reads=[K("xT", d), bkey], writes=[K("xT", d)])

    def ffn(l):
        rmsnorm(lambda c: P("norm_ffn", l, c))
        pend = {"m": None}
        for g in range(6):
            wvg, wkg, n = next_weights(("gate", l))
            wvv, wkv, _ = next_weights(("val", l))
            nch = n // 128
            c0 = g * 4
            halves = [list(range(h0, min(h0 + 2, nch))) for h0 in range(0, nch, 2)]
            for hf, ccs in enumerate(halves):
                lo, hi = ccs[0], ccs[-1] + 1
                S.add("dve", lambda e, lo=lo, hi=hi, c0=c0: e.tensor_copy(out=gbuf[:, lo:hi, 0:2], in_=ghalo[:, l, c0 + lo:c0 + hi, :]),
                      reads=[K("ghalo", l, g, hf), K("gbuf", hf)], writes=[K("gbufh", hf)])
                for cc in ccs:
                    bank, bkey = bank_ffn()
                    proj_fm(wvg, wkg, cc * 128, 128, HK, bank, bkey)
                    act(gbuf[:, cc, 2:TT + 2], bank[:, :], AF.Copy, reads=[bkey, K("gbufh", hf)], writes=[K("gbuf", hf)])
                S.add("dve", lambda e, lo=lo, hi=hi, c0=c0: e.tensor_copy(out=ghalo[:, l, c0 + lo:c0 + hi, :], in_=gbuf[:, lo:hi, TT:TT + 2]),
                      reads=[K("gbuf", hf)], writes=[K("ghalo", l, g, hf)])
            for hf, ccs in enumerate(halves):
                for cc in ccs:
                    c = c0 + cc
                    vb, vbk = bank_ffn()
                    proj_fm(wvv, wkv, cc * 128, 128, HK, vb, vbk)
                    gc, gck = scratch()
                    rk = [K("gbuf", hf), K("gbufh", hf), K("pp")]
                    ts(gc[:], gbuf[:, cc, 2:TT + 2], P("fw2", l, c), P("fb", l, c), ALU.mult, ALU.add, reads=rk, writes=[gck])
                    stt(gc[:], gbuf[:, cc, 1:TT + 1], P("fw1", l, c), gc[:], ALU.mult, ALU.add, reads=rk + [gck], writes=[gck])
                    stt(gc[:], gbuf[:, cc, 0:TT], P("fw0", l, c), gc[:], ALU.mult, ALU.add, reads=rk + [gck], writes=[gck])
                    act(gc[:], gc[:], AF.Gelu_apprx_tanh, reads=[gck], writes=[gck])
                    if pend["m"] is not None:
                        pend["m"]()
                    pend["m"] = (lambda c=c, gc=gc, gck=gck, vb=vb, vbk=vbk:
                                 tt(ybuf[:, c * TT:(c + 1) * TT], gc[:], vb[:, :], ALU.mult, reads=[gck, vbk], writes=[YK[c]]))
        pend["m"]()
        pend["m"] = None
        for d in range(8):
            wv, wk, _ = next_weights(("down", l))
            bank, bkey = bank_ffn()
            mms = [dict(out=bank[:, :], lhsT=wv[:, kc, 0:128], rhs=ybuf[:, kc * TT:(kc + 1) * TT], start=(kc == 0), stop=(kc == NFF - 1)) for kc in range(NFF)]
            mm_group(mms, reads=[wk] + YK, writes=[bkey])
            tt(xT[:, d, :], xT[:, d, :], bank[:, :], ALU.add, reads=[K("xT", d), bkey], writes=[K("xT", d)])

    for t in range(NT):
        load_tile(t)
        for l in range(2):
            dbg_state["on"] = (t == DBG_T and l == 0)
            mixer(l)
            for c in range(8):
                tap(c, oT[:, c, :], [K("oT", c)])
                tap(8 + c, xT[:, c, :], [K("xT", c)])
            ffn(l)
            for c in range(8):
                tap(16 + c, xT[:, c, :], [K("xT", c)])
            dbg_state["on"] = False
        outs = rmsnorm(lambda c: pp[:, G_NF + c:G_NF + c + 1], final=True)
        store_tile(t, outs)
    S.add("sp", None, reads=[K("outd", t * TT + blk * 128) for t in range(NT) for blk in range(4)] + ([K("dbgd", i) for i in range(dbg)] if dbg else []))
    assert wstate["next_use"] == len(stream)
    S.emit(nc, es)
    es.close()
    return nc


_CACHE = {}


def _host_inputs(inp):
    pp = _pack_params(inp)
    cm = _const_mat()
    rgw = np.ascontiguousarray(np.stack([inp["rg_w_a"], inp["rg_w_x"]], axis=1).astype(np.float32))
    com = {
        "w_in": np.ascontiguousarray(inp["w_in"], dtype=np.float32),
        "w_out": np.ascontiguousarray(inp["w_out"], dtype=np.float32),
        "w_gate": np.ascontiguousarray(inp["ffn_w_gate"], dtype=np.float32),
        "w_val": np.ascontiguousarray(inp["ffn_w_val"], dtype=np.float32),
        "w_down": np.ascontiguousarray(inp["ffn_w_down"], dtype=np.float32),
        "pp": pp, "cm": cm, "rgw": rgw,
        "wgup": np.ascontiguousarray(inp["gla_w_gate_up"], dtype=np.float32),
    }
    return com


def run(inp, n_cores=None, dbg=0):
    x = np.asarray(inp["x"], dtype=np.float32)
    B, T, _ = x.shape
    n_cores = B if n_cores is None else n_cores
    nc = build(T, dbg)
    com = _host_inputs(inp)
    in_maps = []
    for b in range(B):
        m = dict(com)
        m["x"] = np.ascontiguousarray(x[b])
        in_maps.append(m)
    res = run_bass_kernel_spmd(nc, in_maps, core_ids=list(range(B)))
    if dbg:
        return np.stack([np.asarray(r["out"], dtype=np.float32) for r in res.results], axis=0), [np.asarray(r["dbg"]) for r in res.results]
    return np.stack([np.asarray(r["out"], dtype=np.float32) for r in res.results], axis=0)


def kernel(**inputs):
    inp = {k: np.asarray(v) for k, v in inputs.items()}
    return run(inp)
```
